# Optimizing a Trainium2 kernel written in Bass

```python
import jax, jax.numpy as jnp
from jax import lax
import numpy as np

D_MODEL = 2048
BATCH = 2
SEQ = 8192
DEPTH = 1

MLA_HEADS = 16
Q_LORA = 512
KV_LORA = 512
QK_NOPE = 128
QK_ROPE = 64
V_HEAD = 128
QK_HEAD = QK_NOPE + QK_ROPE
ROPE_THETA = 10000.0
Q_BLOCK = 128
SGU_WIDTH = D_MODEL
SGU_GROUPS = 8
SGU_GROUP_DIM = SGU_WIDTH // SGU_GROUPS
CHUNK = 128
PEER_HEADS = 8
N_KEYS = 128
N_EXPERTS = N_KEYS * N_KEYS
PEER_TOPK = 16
KEY_DIM = 128
TOKEN_BLOCK = 128
EPS = 1e-6

OFF_CQ = Q_LORA
OFF_CKV = OFF_CQ + KV_LORA
OFF_KPE = OFF_CKV + QK_ROPE
OFF_SGU = OFF_KPE + 2 * SGU_WIDTH
IN_COLS = OFF_SGU + 2 * D_MODEL

kernel_name = "hybrid_mla_sgu_peer_block"


def rmsnorm(x, g):
    xf = x.astype(jnp.float32)
    r = lax.rsqrt(jnp.mean(xf * xf, axis=-1, keepdims=True) + EPS)
    return (xf * r).astype(x.dtype) * g


def layernorm(x, g, b):
    xf = x.astype(jnp.float32)
    mu = jnp.mean(xf, axis=-1, keepdims=True)
    var = jnp.mean(jnp.square(xf - mu), axis=-1, keepdims=True)
    return ((xf - mu) * lax.rsqrt(var + EPS)).astype(x.dtype) * g + b


def rope(x, positions):
    d = x.shape[-1]
    freqs = ROPE_THETA ** (-jnp.arange(0, d, 2, dtype=jnp.float32) / d)
    ang = positions.astype(jnp.float32)[..., None] * freqs
    cos = jnp.cos(ang)[:, :, None, :].astype(x.dtype)
    sin = jnp.sin(ang)[:, :, None, :].astype(x.dtype)
    x1, x2 = x[..., : d // 2], x[..., d // 2 :]
    return jnp.concatenate([x1 * cos - x2 * sin, x1 * sin + x2 * cos], axis=-1)


def causal_block_attention(q, k, v):
    B, S, H, Dq = q.shape
    Dv = v.shape[-1]
    nb = S // Q_BLOCK
    qb = q.reshape(B, nb, Q_BLOCK, H, Dq).transpose(1, 0, 2, 3, 4)
    key_pos = jnp.arange(S)
    scale = Dq ** -0.5

    def one_block(args):
        q_blk, blk = args
        s = jnp.einsum("bqhd,bkhd->bhqk", q_blk, k).astype(jnp.float32) * scale
        q_pos = blk * Q_BLOCK + jnp.arange(Q_BLOCK)
        mask = q_pos[:, None] >= key_pos[None, :]
        s = jnp.where(mask[None, None], s, -jnp.inf)
        p = jax.nn.softmax(s, axis=-1).astype(v.dtype)
        return jnp.einsum("bhqk,bkhd->bqhd", p, v)

    o = lax.map(one_block, (qb, jnp.arange(nb)))
    return o.transpose(1, 0, 2, 3, 4).reshape(B, S, H * Dv)


def mla_branch(c_q, c_kv, k_pe, positions, g_q_a, w_uq, g_kv_a, w_ukv):
    B, S, _ = c_q.shape
    q = jnp.einsum("bsr,rhd->bshd", rmsnorm(c_q, g_q_a), w_uq)
    q = jnp.concatenate([q[..., :QK_NOPE], rope(q[..., QK_NOPE:], positions)], axis=-1)
    kv = jnp.einsum("bsr,rhd->bshd", rmsnorm(c_kv, g_kv_a), w_ukv)
    k_nope, v = kv[..., :QK_NOPE], kv[..., QK_NOPE:]
    k_pe = rope(k_pe[:, :, None, :], positions)
    k = jnp.concatenate([k_nope, jnp.broadcast_to(k_pe, (B, S, MLA_HEADS, QK_ROPE))], axis=-1)
    return causal_block_attention(q, k, v)


def sgu_branch(z, ln_g, ln_b, w_spatial, b_spatial):
    z = jax.nn.gelu(z)
    u, v = z[..., :SGU_WIDTH], z[..., SGU_WIDTH:]
    v = layernorm(v, ln_g, ln_b)
    B, S, _ = v.shape
    vg = v.reshape(B, S // CHUNK, CHUNK, SGU_GROUPS, SGU_GROUP_DIM)
    w = w_spatial * jnp.tril(jnp.ones((CHUNK, CHUNK), dtype=w_spatial.dtype))[None]
    mixed = jnp.einsum("gts,bcsgd->bctgd", w, vg) + b_spatial.T[None, None, :, :, None]
    return u * mixed.reshape(B, S, SGU_WIDTH)


def peer_ffn(h, w_pq, peer_keys, peer_u, peer_v):
    B, S, D = h.shape
    q = jnp.einsum("bsd,dhk->bshk", h, w_pq).reshape(B, S, PEER_HEADS, 2, KEY_DIM)
    sub = jnp.einsum("bshpk,hpnk->bshpn", q, peer_keys).astype(jnp.float32)
    s, i = lax.top_k(sub, PEER_TOPK)
    cand = (s[..., 0, :, None] + s[..., 1, None, :]).reshape(B, S, PEER_HEADS, PEER_TOPK * PEER_TOPK)
    cidx = (i[..., 0, :, None] * N_KEYS + i[..., 1, None, :]).reshape(B, S, PEER_HEADS, PEER_TOPK * PEER_TOPK)
    top_s, pos = lax.top_k(cand, PEER_TOPK)
    idx = jnp.take_along_axis(cidx, pos, axis=-1)
    gate = jax.nn.softmax(top_s, axis=-1).astype(h.dtype)
    nt = (B * S) // TOKEN_BLOCK
    hb = h.reshape(nt, TOKEN_BLOCK, D)
    ib = idx.reshape(nt, TOKEN_BLOCK, PEER_HEADS, PEER_TOPK)
    gb = gate.reshape(nt, TOKEN_BLOCK, PEER_HEADS, PEER_TOPK)

    def one_block(args):
        x_blk, i_blk, g_blk = args
        u = jnp.take(peer_u, i_blk, axis=0)
        a = jax.nn.gelu(jnp.einsum("td,thkd->thk", x_blk, u)) * g_blk
        v = jnp.take(peer_v, i_blk, axis=0)
        return jnp.einsum("thk,thkd->td", a, v)

    return lax.map(one_block, (hb, ib, gb)).reshape(B, S, D)


def setup_inputs(seed: int = 0) -> dict:
    key = jax.random.key(seed)
    ks = jax.random.split(key, 24)
    f32 = jnp.float32
    nrm = lambda k, shape, scale: jax.random.normal(k, shape, f32) * scale
    gain = lambda k, shape: 1.0 + 0.05 * jax.random.normal(k, shape, f32)
    L = DEPTH
    x = jax.random.normal(ks[0], (BATCH, SEQ, D_MODEL), f32)
    offsets = jax.random.randint(ks[1], (BATCH, 1), 0, 4096, dtype=jnp.int32)
    positions = offsets + jnp.arange(SEQ, dtype=jnp.int32)[None, :]
    return {
        "x": x,
        "positions": positions,
        "g_norm1": gain(ks[2], (L, D_MODEL)),
        "w_in": nrm(ks[3], (L, D_MODEL, IN_COLS), D_MODEL ** -0.5),
        "g_q_a": gain(ks[4], (L, Q_LORA)),
        "w_uq": nrm(ks[5], (L, Q_LORA, MLA_HEADS, QK_HEAD), Q_LORA ** -0.5),
        "g_kv_a": gain(ks[6], (L, KV_LORA)),
        "w_ukv": nrm(ks[7], (L, KV_LORA, MLA_HEADS, QK_NOPE + V_HEAD), KV_LORA ** -0.5),
        "sgu_ln_g": gain(ks[8], (L, SGU_WIDTH)),
        "sgu_ln_b": nrm(ks[9], (L, SGU_WIDTH), 0.02),
        "w_spatial": nrm(ks[10], (L, SGU_GROUPS, CHUNK, CHUNK), CHUNK ** -0.5),
        "b_spatial": gain(ks[11], (L, SGU_GROUPS, CHUNK)),
        "b_gate": nrm(ks[12], (L, 2 * D_MODEL), 0.01),
        "w_out": nrm(ks[13], (L, D_MODEL, D_MODEL), D_MODEL ** -0.5),
        "g_norm2": gain(ks[14], (L, D_MODEL)),
        "w_peer_q": nrm(ks[15], (L, D_MODEL, PEER_HEADS, 2 * KEY_DIM), D_MODEL ** -0.5),
        "peer_keys": nrm(ks[16], (L, PEER_HEADS, 2, N_KEYS, KEY_DIM), KEY_DIM ** -0.5),
        "peer_u": nrm(ks[17], (L, N_EXPERTS, D_MODEL), D_MODEL ** -0.5),
        "peer_v": nrm(ks[18], (L, N_EXPERTS, D_MODEL), PEER_HEADS ** -0.5),
        "g_final": gain(ks[19], (D_MODEL,)),
    }


def reference(x, positions, g_norm1, w_in, g_q_a, w_uq, g_kv_a, w_ukv, sgu_ln_g, sgu_ln_b,
              w_spatial, b_spatial, b_gate, w_out, g_norm2, w_peer_q, peer_keys, peer_u,
              peer_v, g_final):
    for l in range(DEPTH):
        h = rmsnorm(x, g_norm1[l])
        proj = h @ w_in[l]
        c_q = proj[..., :OFF_CQ]
        c_kv = proj[..., OFF_CQ:OFF_CKV]
        k_pe = proj[..., OFF_CKV:OFF_KPE]
        z_sgu = proj[..., OFF_KPE:OFF_SGU]
        gates = jax.nn.sigmoid(proj[..., OFF_SGU:] + b_gate[l])
        o_a = mla_branch(c_q, c_kv, k_pe, positions, g_q_a[l], w_uq[l], g_kv_a[l], w_ukv[l])
        o_b = sgu_branch(z_sgu, sgu_ln_g[l], sgu_ln_b[l], w_spatial[l], b_spatial[l])
        merged = gates[..., :D_MODEL] * o_a + gates[..., D_MODEL:] * o_b
        x = x + merged @ w_out[l]
        h2 = rmsnorm(x, g_norm2[l])
        x = x + peer_ffn(h2, w_peer_q[l], peer_keys[l], peer_u[l], peer_v[l])
    return rmsnorm(x, g_final)
```

```python
import math
from contextlib import ExitStack

import numpy as np
import concourse.bass as bass
import concourse.mybir as mybir
from concourse.bass_utils import run_bass_kernel_spmd

F32 = mybir.dt.float32
BF16 = mybir.dt.bfloat16
U32 = mybir.dt.uint32
I32 = mybir.dt.int32
AF = mybir.ActivationFunctionType
ALU = mybir.AluOpType
AX = mybir.AxisListType

EPS = 1e-6
SEM_LIMIT = 24000
NBA = 64
NOWN = 16
NH = 16
QSCALE = 192.0 ** -0.5
TWO_PI = 2.0 * math.pi
C1 = 6.28125
C2 = TWO_PI - C1
PI_SAFE = 3.1415920


class Buf:
    __slots__ = ("name", "lw", "rd", "dsem", "dcnt")

    def __init__(self, name):
        self.name = name
        self.lw = []
        self.rd = []
        self.dsem = None
        self.dcnt = 0


def _compact(evs):
    best = {}
    for s, v in evs:
        k = id(s)
        if k not in best or best[k][1] < v:
            best[k] = (s, v)
    return list(best.values())


class Em:
    def __init__(self, nc, stack):
        self.nc = nc
        self.stack = stack
        self.engs = {"pe": nc.tensor, "act": nc.scalar, "dve": nc.vector, "pool": nc.gpsimd, "sp": nc.sync}
        self.sem = {}
        self.cnt = {}
        self.nsem = 0
        for k in self.engs:
            self.sem[k] = self._newsem("s_" + k)
            self.cnt[k] = 0
        self.waited = {k: {} for k in self.engs}
        self.dbufs = []
        self.old = []
        self.ninst = 0

    def _newsem(self, name):
        self.nsem += 1
        return self.stack.enter_context(self.nc.semaphore(f"{name}_{self.nsem}"))

    def buf(self, name):
        return Buf(name)

    def bufs(self, name, n):
        return [Buf(f"{name}{i}") for i in range(n)]

    def _wait(self, eng, events):
        w = self.waited[eng]
        best = {}
        for (s, v) in events:
            k = id(s)
            if w.get(k, 0) < v and (k not in best or best[k][1] < v):
                best[k] = (s, v)
        for k, (s, v) in best.items():
            self.engs[eng].wait_ge(s, v)
            w[k] = v

    def op(self, eng, fn, reads=(), writes=(), acc=False):
        ev = []
        for b in reads:
            ev.extend(b.lw)
        for b in writes:
            ev.extend(b.rd)
            ev.extend(b.lw)
        if eng == "pe":
            mine = id(self.sem["pe"])
            ev = [e for e in ev if id(e[0]) != mine]
        self._wait(eng, ev)
        if self.cnt[eng] >= SEM_LIMIT:
            self.old.append((self.sem[eng], self.cnt[eng]))
            self.sem[eng] = self._newsem("s_" + eng)
            self.cnt[eng] = 0
        ins = fn(self.engs[eng])
        self.cnt[eng] += 1
        ins.then_inc(self.sem[eng], 1)
        e = (self.sem[eng], self.cnt[eng])
        for b in reads:
            b.rd.append(e)
            if len(b.rd) > 16:
                b.rd = _compact(b.rd)
        for b in writes:
            if acc:
                b.lw.append(e)
                if len(b.lw) > 16:
                    b.lw = _compact(b.lw)
            else:
                b.lw = [e]
                b.rd = []
        self.ninst += 1
        return ins

    def dma(self, q, fn, reads=(), writes=(), sembuf=None, group=False):
        sb = sembuf or (writes[0] if writes else reads[0])
        ev = []
        for b in reads:
            ev.extend(b.lw)
        for b in writes:
            ev.extend(b.rd)
            if not group:
                ev.extend(b.lw)
        self._wait(q, ev)
        if sb.dsem is None or sb.dcnt >= SEM_LIMIT:
            if sb.dsem is not None:
                self.old.append((sb.dsem, sb.dcnt))
            else:
                self.dbufs.append(sb)
            sb.dsem = self._newsem("d_" + sb.name)
            sb.dcnt = 0
        ins = fn(self.engs[q])
        sb.dcnt += 16
        ins.then_inc(sb.dsem, 16)
        e = (sb.dsem, sb.dcnt)
        for b in reads:
            b.rd.append(e)
            if len(b.rd) > 16:
                b.rd = _compact(b.rd)
        for b in writes:
            if group:
                b.lw.append(e)
                if len(b.lw) > 16:
                    b.lw = _compact(b.lw)
            else:
                b.lw = [e]
                b.rd = []
        self.ninst += 1
        return ins

    def all_events(self):
        ev = [(self.sem[k], self.cnt[k]) for k in self.engs if self.cnt[k] > 0]
        ev += [(b.dsem, b.dcnt) for b in self.dbufs if b.dcnt > 0]
        ev += self.old
        return ev

    def barrier(self, engines=("pe", "act", "dve", "pool", "sp")):
        ev = self.all_events()
        for e in engines:
            self._wait(e, ev)
        self.old = []


def build_nc(stop=None, dbg=False):
    nc = bass.Bass("TRN2", target_bir_lowering=False)

    def din(name, shape, dt=F32):
        return nc.dram_tensor(name, shape, dt, kind="ExternalInput").ap()

    xb = din("xb", [NBA * 128, 2048])
    xo = din("xo", [NOWN * 128, 2048])
    posb = din("posb", [128, NBA], I32)
    poso = din("poso", [128, NOWN], I32)
    cmask_d = din("cmask", [128, 512])
    freqs_d = din("freqs", [128, 32])
    ident_d = din("ident", [128, 128])
    iota4_d = din("iota4", [128, 2048])
    g1_d = din("g1", [128, 16])
    w_in = din("w_in", [2048, 9280])
    gq_d = din("gq", [128, 4])
    gkv_d = din("gkv", [128, 4])
    w_uq = din("w_uq", [512, 3072])
    w_ukv = din("w_ukv", [512, 4096])
    lng_d = din("lng", [128, 2048])
    lnb_d = din("lnb", [128, 2048])
    wspT_d = din("wspT", [128, 8, 128])
    triT_d = din("triT", [128, 128])
    bsp_d = din("bsp", [128, 8])
    bgate_d = din("bgate", [128, 4096])
    w_out = din("w_out", [2048, 2048])
    g2_d = din("g2b", [128, 2048])
    w_pq = din("w_pq", [2048, 2048])
    keysT_d = din("keysT", [128, 16, 128])
    peer_u = din("peer_u", [16384, 2048])
    peer_v = din("peer_v", [16384, 2048])
    gf_d = din("gfb", [128, 2048])
    y = nc.dram_tensor("y", [NOWN * 128, 2048], F32, kind="ExternalOutput").ap()

    skind = "ExternalOutput" if dbg else "Internal"
    zs = nc.dram_tensor("zs", [NOWN * 128, 4096], BF16, kind=skind).ap()
    gs = nc.dram_tensor("gs", [NOWN * 128, 4096], BF16, kind=skind).ap()
    qrs = nc.dram_tensor("qrs", [128, 8, NOWN * 128], BF16, kind=skind).ap()
    oas = nc.dram_tensor("oas", [NOWN * 128, 2048], BF16, kind=skind).ap()
    x1s = nc.dram_tensor("x1s", [NOWN * 128, 2048], F32, kind=skind).ap()
    ub = nc.dram_tensor("ub", [16384, 2048], BF16).ap()
    vb = nc.dram_tensor("vb", [16384, 2048], BF16).ap()
    if dbg:
        d_cqnT = nc.dram_tensor("d_cqnT", [128, 4, NOWN * 128], BF16, kind="ExternalOutput").ap()
        d_ckvnT = nc.dram_tensor("d_ckvnT", [128, 4, NBA * 128], BF16, kind="ExternalOutput").ap()
        d_kpeT = nc.dram_tensor("d_kpeT", [128, NBA * 128], BF16, kind="ExternalOutput").ap()

    with ExitStack() as gst:
        em = Em(nc, gst)
        b_ub = em.buf("ub")
        b_vb = em.buf("vb")
        tab_jobs = [(peer_u, ub, b_ub, i) for i in range(32)] + [(peer_v, vb, b_vb, i) for i in range(32)]

        def emit_tab_jobs(n):
            for _ in range(n):
                if not tab_jobs:
                    return
                src, dst, bb, i = tab_jobs.pop(0)
                em.dma("pool", lambda e: e.dma_start(out=dst[i * 512:(i + 1) * 512, :], in_=src[i * 512:(i + 1) * 512, :]), writes=[bb], group=True)

        def T(st, name, shape, dt):
            return st.enter_context(nc.sbuf_tensor("sb_" + name, shape, dt))

        def P(st, name, shape, dt=F32):
            return st.enter_context(nc.psum_tensor("ps_" + name, shape, dt))

        identf = T(gst, "identf", [128, 128], F32)
        identb = T(gst, "identb", [128, 128], BF16)
        b_identf = em.buf("identf")
        b_identb = em.buf("identb")
        em.dma("sp", lambda e: e.dma_start(out=identf[:], in_=ident_d[:, :]), writes=[b_identf])
        em.op("dve", lambda e: e.tensor_copy(out=identb[:], in_=identf[:]), reads=[b_identf], writes=[b_identb])

        def load_small(st, name, src, shape, dt=F32):
            t = T(st, name, shape, dt)
            b = em.buf(name)
            em.dma("sp", lambda e: e.dma_start(out=t[:], in_=src), writes=[b])
            return t, b


        def rstd_chain(st_t, b_st, c_ss, c0, n):
            em.op("dve", lambda e: e.tensor_scalar(out=st_t[:, c0:c0 + 1], in0=st_t[:, c_ss:c_ss + 1], scalar1=1.0 / n,
                                                   scalar2=EPS, op0=ALU.mult, op1=ALU.add), reads=[b_st], writes=[b_st])
            em.op("act", lambda e: e.activation(out=st_t[:, c0 + 1:c0 + 2], in_=st_t[:, c0:c0 + 1], func=AF.Sqrt),
                  reads=[b_st], writes=[b_st])
            em.op("dve", lambda e: e.reciprocal(out=st_t[:, c0 + 2:c0 + 3], in_=st_t[:, c0 + 1:c0 + 2]),
                  reads=[b_st], writes=[b_st])
            return c0 + 2

        def sincos_tables(st, name, pos_src, nblk, scale, want_b=True):
            n = nblk * 64
            csA = T(st, name + "_csA", [128, nblk, 64], F32)
            csB = T(st, name + "_csB", [128, nblk, 64], F32) if want_b else None
            b = em.buf(name + "_cs")
            with ExitStack() as tmp:
                posi, b_posi = load_small(tmp, name + "_pi", pos_src, [128, nblk], I32)
                frq, b_frq = load_small(tmp, name + "_fr", freqs_d[:, :], [128, 32])
                posf = T(tmp, name + "_pf", [128, nblk], F32)
                A2 = T(tmp, name + "_A2", [128, nblk, 64], F32)
                kf = T(tmp, name + "_kf", [128, n], F32)
                ki = T(tmp, name + "_ki", [128, n], I32)
                A2f = A2[:].rearrange("p b e -> p (b e)")
                csAf = csA[:].rearrange("p b e -> p (b e)")
                em.op("dve", lambda e: e.tensor_copy(out=posf[:], in_=posi[:]), reads=[b_posi], writes=[b])
                em.op("dve", lambda e: e.tensor_tensor(out=A2[:, :, 32:64], in0=posf[:].unsqueeze(2).to_broadcast([128, nblk, 32]),
                                                       in1=frq[:].unsqueeze(1).to_broadcast([128, nblk, 32]), op=ALU.mult),
                      reads=[b, b_frq], writes=[b])
                em.op("dve", lambda e: e.tensor_scalar(out=A2[:, :, 0:32], in0=A2[:, :, 32:64], scalar1=math.pi / 2, scalar2=None,
                                                       op0=ALU.add), reads=[b], writes=[b])
                em.op("dve", lambda e: e.tensor_scalar(out=kf[:], in0=A2f, scalar1=1.0 / TWO_PI, scalar2=None, op0=ALU.mult),
                      reads=[b], writes=[b])
                em.op("dve", lambda e: e.tensor_copy(out=ki[:], in_=kf[:]), reads=[b], writes=[b])
                em.op("dve", lambda e: e.tensor_copy(out=kf[:], in_=ki[:]), reads=[b], writes=[b])
                em.op("dve", lambda e: e.scalar_tensor_tensor(out=A2f, in0=kf[:], scalar=-C1, in1=A2f, op0=ALU.mult, op1=ALU.add),
                      reads=[b], writes=[b])
                em.op("dve", lambda e: e.scalar_tensor_tensor(out=A2f, in0=kf[:], scalar=-C2, in1=A2f, op0=ALU.mult, op1=ALU.add),
                      reads=[b], writes=[b])
                em.op("dve", lambda e: e.tensor_scalar(out=kf[:], in0=A2f, scalar1=math.pi, scalar2=None, op0=ALU.is_gt),
                      reads=[b], writes=[b])
                em.op("dve", lambda e: e.scalar_tensor_tensor(out=A2f, in0=kf[:], scalar=-TWO_PI, in1=A2f, op0=ALU.mult, op1=ALU.add),
                      reads=[b], writes=[b])
                em.op("dve", lambda e: e.tensor_scalar(out=kf[:], in0=A2f, scalar1=-math.pi, scalar2=None, op0=ALU.is_lt),
                      reads=[b], writes=[b])
                em.op("dve", lambda e: e.scalar_tensor_tensor(out=A2f, in0=kf[:], scalar=TWO_PI, in1=A2f, op0=ALU.mult, op1=ALU.add),
                      reads=[b], writes=[b])
                em.op("dve", lambda e: e.tensor_scalar(out=A2f, in0=A2f, scalar1=PI_SAFE, scalar2=-PI_SAFE, op0=ALU.min, op1=ALU.max),
                      reads=[b], writes=[b])
                em.op("act", lambda e: e.activation(out=csAf, in_=A2f, func=AF.Sin), reads=[b], writes=[b])
                if scale != 1.0:
                    em.op("dve", lambda e: e.tensor_scalar(out=csAf, in0=csAf, scalar1=scale, scalar2=None, op0=ALU.mult),
                          reads=[b], writes=[b])
                if want_b:
                    em.op("dve", lambda e: e.tensor_copy(out=csB[:, :, 0:32], in_=csA[:, :, 32:64]), reads=[b], writes=[b])
                    em.op("dve", lambda e: e.tensor_copy(out=csB[:, :, 32:64], in_=csA[:, :, 0:32]), reads=[b], writes=[b])
                em.barrier()
            return csA, csB, b

        with ExitStack() as phAB:
            cqnT = T(phAB, "cqnT", [128, 4, NOWN * 128], BF16)
            b_cqnT = em.bufs("cqnT", NOWN)
            with ExitStack() as ph:
                g1, b_g1 = load_small(ph, "g1", g1_d[:, :], [128, 16])
                gq, b_gq = load_small(ph, "gq", gq_d[:, :], [128, 4])
                csAo, csBo, b_cso = sincos_tables(ph, "cso", poso[:, :], NOWN, QSCALE)

                hT = T(ph, "hT_own", [128, 16, NOWN * 128], BF16)
                b_hT = em.bufs("hTo", NOWN)
                stt = [T(ph, f"Ast{i}", [128, 8], F32) for i in range(2)]
                b_stt = em.bufs("Ast", 2)
                junk = T(ph, "Ajunk", [128, 2048], BF16)
                b_junk = em.buf("Ajunk")
                tpp = P(ph, "Atp", [128, 16, 128], BF16)
                b_tpp = em.buf("Atp")
                pacc = [P(ph, f"Apacc{i}", [128, 512], F32) for i in range(2)]
                b_pacc = em.bufs("Apacc", 2)
                pqr = P(ph, "Apqr", [128, 1024], F32)
                b_pqr = em.buf("Apqr")

                with ExitStack() as a0:
                    xt = [T(a0, f"Ax{i}", [128, 2048], F32) for i in range(2)]
                    b_xt = em.bufs("Ax", 2)
                    xs = [T(a0, f"Axs{i}", [128, 2048], BF16) for i in range(2)]
                    b_xs = em.bufs("Axs", 2)
                    for m in range(NOWN):
                        i = m % 2
                        em.dma("sp", lambda e: e.dma_start(out=xt[i][:], in_=xo[m * 128:(m + 1) * 128, :]), writes=[b_xt[i]])
                        em.op("dve", lambda e: e.memset(stt[i][:], 0.0), writes=[b_stt[i]])
                        em.op("dve", lambda e: e.scalar_tensor_tensor(out=junk[:], in0=xt[i][:], scalar=1.0, in1=xt[i][:], op0=ALU.mult,
                                                                       op1=ALU.mult, accum_out=stt[i][:, 0:1]),
                              reads=[b_xt[i]], writes=[b_junk, b_stt[i]])
                        cr = rstd_chain(stt[i], b_stt[i], 0, 1, 2048)
                        em.op("dve", lambda e: e.tensor_scalar(out=xs[i][:], in0=xt[i][:], scalar1=stt[i][:, cr:cr + 1], scalar2=None, op0=ALU.mult),
                              reads=[b_xt[i], b_stt[i]], writes=[b_xs[i]])
                        for kc in range(16):
                            em.op("pe", lambda e: e.transpose(out=tpp[:, kc, :], in_=xs[i][:, kc * 128:(kc + 1) * 128], identity=identb[:]),
                                  reads=[b_xs[i], b_identb], writes=[b_tpp], acc=(kc > 0))
                        em.op("dve", lambda e: e.tensor_tensor(out=hT[:, :, m * 128:(m + 1) * 128], in0=tpp[:],
                                                               in1=g1[:].unsqueeze(2).to_broadcast([128, 16, 128]), op=ALU.mult),
                              reads=[b_tpp, b_g1], writes=[b_hT[m]])
                    em.barrier()

                a1 = ph
                wst = T(a1, "Awst", [128, 16, 512], F32)
                b_wst = em.buf("Awst")
                wbf = [T(a1, f"Awbf{i}", [128, 16, 512], BF16) for i in range(2)]
                b_wbf = em.bufs("Awbf", 2)
                ost = [T(a1, f"Aost{i}", [128, 512], BF16) for i in range(3)]
                b_ost = em.bufs("Aost", 3)
                gtmp = [T(a1, f"Agt{i}", [128, 512], F32) for i in range(2)]
                b_gtmp = em.bufs("Agt", 2)
                bgt = [T(a1, f"Abg{i}", [128, 512], F32) for i in range(2)]
                b_bgt = em.bufs("Abg", 2)

                chunks = [("cq", 0)] + [("z", 1088 + 512 * i) for i in range(8)] + [("g", 5184 + 512 * i) for i in range(8)]
                cvt_engs = ["dve", "pool"]

                def load_chunk_dma(ci):
                    kind, c0 = chunks[ci]
                    em.dma("sp", lambda e: e.dma_start(out=wst[:], in_=w_in[:, c0:c0 + 512].rearrange("(kc p) n -> p kc n", p=128)),
                           writes=[b_wst])
                    if kind == "g":
                        gc = c0 - 5184
                        em.dma("sp", lambda e: e.dma_start(out=bgt[ci % 2][:], in_=bgate_d[:, gc:gc + 512]), writes=[b_bgt[ci % 2]])

                def load_chunk_cvt(ci):
                    wb = wbf[ci % 2]
                    for hh in range(2):
                        eng = cvt_engs[hh]
                        em.op(eng, lambda e: e.tensor_copy(out=wb[:, hh * 8:(hh + 1) * 8, :], in_=wst[:, hh * 8:(hh + 1) * 8, :]),
                              reads=[b_wst], writes=[b_wbf[ci % 2]], acc=(hh > 0))

                load_chunk_dma(0)
                load_chunk_cvt(0)
                oi = 0
                gi = 0
                pi_ = 0
                for ci, (kind, c0) in enumerate(chunks):
                    if ci + 1 < len(chunks):
                        load_chunk_dma(ci + 1)
                    wb = wbf[ci % 2]
                    b_wb = b_wbf[ci % 2]
                    for m in range(NOWN):
                        if m == 8 and ci + 1 < len(chunks):
                            load_chunk_cvt(ci + 1)
                        pa = pacc[pi_ % 2]
                        b_pa = b_pacc[pi_ % 2]
                        pi_ += 1
                        for kc in range(16):
                            em.op("pe", lambda e: e.matmul(pa[:], lhsT=hT[:, kc, m * 128:(m + 1) * 128], rhs=wb[:, kc, :],
                                                           start=(kc == 0), stop=(kc == 15)),
                                  reads=[b_hT[m], b_wb], writes=[b_pa], acc=(kc > 0))
                        if kind == "cq":
                            i = m % 2
                            em.op("act", lambda e: e.activation(out=junk[:, 0:512], in_=pa[:], func=AF.Square, accum_out=stt[i][:, 4:5]),
                                  reads=[b_pa], writes=[b_junk, b_stt[i]])
                            cr = rstd_chain(stt[i], b_stt[i], 4, 5, 512)
                            o = ost[oi % 3]
                            b_o = b_ost[oi % 3]
                            oi += 1
                            em.op("dve", lambda e: e.tensor_scalar(out=o[:], in0=pa[:], scalar1=stt[i][:, cr:cr + 1], scalar2=None, op0=ALU.mult),
                                  reads=[b_pa, b_stt[i]], writes=[b_o])
                            for rc in range(4):
                                em.op("pe", lambda e: e.transpose(out=tpp[:, rc, :], in_=o[:, rc * 128:(rc + 1) * 128], identity=identb[:]),
                                      reads=[b_o, b_identb], writes=[b_tpp], acc=(rc > 0))
                            em.op("dve", lambda e: e.tensor_tensor(out=cqnT[:, :, m * 128:(m + 1) * 128], in0=tpp[:, 0:4, :],
                                                                   in1=gq[:].unsqueeze(2).to_broadcast([128, 4, 128]), op=ALU.mult),
                                  reads=[b_tpp, b_gq], writes=[b_cqnT[m]])
                        elif kind == "z":
                            o = ost[oi % 3]
                            b_o = b_ost[oi % 3]
                            oi += 1
                            em.op("act", lambda e: e.activation(out=o[:], in_=pa[:], func=AF.Gelu), reads=[b_pa], writes=[b_o])
                            zc = c0 - 1088
                            em.dma("pool", lambda e: e.dma_start(out=zs[m * 128:(m + 1) * 128, zc:zc + 512], in_=o[:]), reads=[b_o])
                        else:
                            gc = c0 - 5184
                            gt = gtmp[gi % 2]
                            b_gt = b_gtmp[gi % 2]
                            gi += 1
                            em.op("dve", lambda e: e.tensor_tensor(out=gt[:], in0=pa[:], in1=bgt[ci % 2][:], op=ALU.add),
                                  reads=[b_pa, b_bgt[ci % 2]], writes=[b_gt])
                            o = ost[oi % 3]
                            b_o = b_ost[oi % 3]
                            oi += 1
                            em.op("act", lambda e: e.activation(out=o[:], in_=gt[:], func=AF.Sigmoid), reads=[b_gt], writes=[b_o])
                            em.dma("pool", lambda e: e.dma_start(out=gs[m * 128:(m + 1) * 128, gc:gc + 512], in_=o[:]), reads=[b_o])

                wqr_st = wst[:, 0:8, :].rearrange("p r n -> p (r n)").rearrange("p (r h e) -> p r h e", r=4, h=16)
                wqr = wbf[0][:, 0:8, :].rearrange("p r n -> p (r n)").rearrange("p (r h e) -> p r h e", r=4, h=16)
                w_uq_v = w_uq.rearrange("(rc p) (h e) -> p rc h e", p=128, e=192)
                for rc in range(4):
                    em.dma("sp", lambda e: e.dma_start(out=wqr_st[:, rc, :, :], in_=w_uq_v[:, rc, :, 128:192]), writes=[b_wst],
                           group=(rc > 0))
                em.op("dve", lambda e: e.tensor_copy(out=wqr, in_=wqr_st), reads=[b_wst], writes=[b_wbf[0]])
                wqr2 = wbf[0][:, 0:8, :].rearrange("p r n -> p (r n)").rearrange("p (r n) -> p r n", r=4)
                ra = T(ph, "Ara", [128, 16, 64], F32)
                rb = T(ph, "Arb", [128, 16, 64], F32)
                b_rab = em.buf("Arab")
                qr = [T(ph, f"Aqr{i}", [128, 16, 64], BF16) for i in range(2)]
                b_qr = em.bufs("Aqr", 2)
                qrT = [T(ph, f"AqrT{i}", [128, 8, 128], BF16) for i in range(2)]
                b_qrT = em.bufs("AqrT", 2)
                pqr3 = pqr[:].rearrange("p (h e) -> p h e", e=64)
                for m in range(NOWN):
                    i = m % 2
                    for half in range(2):
                        for rc in range(4):
                            em.op("pe", lambda e: e.matmul(pqr[:, half * 512:(half + 1) * 512], lhsT=cqnT[:, rc, m * 128:(m + 1) * 128],
                                                           rhs=wqr2[:, rc, half * 512:(half + 1) * 512], start=(rc == 0), stop=(rc == 3)),
                                  reads=[b_cqnT[m], b_wbf[0]], writes=[b_pqr], acc=not (half == 0 and rc == 0))
                    cA = csAo[:, m, :].unsqueeze(1).to_broadcast([128, 16, 64])
                    cB = csBo[:, m, :].unsqueeze(1).to_broadcast([128, 16, 64])
                    em.op("dve", lambda e: e.tensor_tensor(out=ra[:], in0=pqr3, in1=cA, op=ALU.mult), reads=[b_pqr, b_cso], writes=[b_rab])
                    em.op("dve", lambda e: e.tensor_tensor(out=rb[:], in0=pqr3, in1=cB, op=ALU.mult), reads=[b_pqr, b_cso], writes=[b_rab],
                          acc=True)
                    em.op("dve", lambda e: e.tensor_tensor(out=qr[i][:, :, 0:32], in0=ra[:, :, 0:32], in1=ra[:, :, 32:64], op=ALU.subtract),
                          reads=[b_rab], writes=[b_qr[i]])
                    em.op("dve", lambda e: e.tensor_tensor(out=qr[i][:, :, 32:64], in0=rb[:, :, 0:32], in1=rb[:, :, 32:64], op=ALU.add),
                          reads=[b_rab], writes=[b_qr[i]], acc=True)
                    qrf = qr[i][:].rearrange("p h e -> p (h e)")
                    for pr in range(8):
                        em.op("pe", lambda e: e.transpose(out=tpp[:, pr, :], in_=qrf[:, pr * 128:(pr + 1) * 128], identity=identb[:]),
                              reads=[b_qr[i], b_identb], writes=[b_tpp], acc=(pr > 0))
                    em.op("act", lambda e: e.activation(out=qrT[i][:], in_=tpp[:, 0:8, :], func=AF.Copy), reads=[b_tpp], writes=[b_qrT[i]])
                    em.dma("pool", lambda e: e.dma_start(out=qrs[:, :, m * 128:(m + 1) * 128], in_=qrT[i][:]), reads=[b_qrT[i]])
                em.barrier()
            if dbg:
                em.dma("sp", lambda e: e.dma_start(out=d_cqnT[:, :, :], in_=cqnT[:]), reads=list(b_cqnT))
            if stop == "A":
                em.barrier()
                return nc

            with ExitStack() as phLB:
                ckvnT = T(phLB, "ckvnT", [128, NBA, 4, 128], BF16)
                b_ckvnT = em.bufs("ckvnT", NBA)
                kpeT = T(phLB, "kpeT", [128, NBA * 128], BF16)
                b_kpeT = em.bufs("kpeT", NBA)

                with ExitStack() as ph:
                    g1, b_g1 = load_small(ph, "Lg1", g1_d[:, :], [128, 16])
                    gkv, b_gkv = load_small(ph, "Lgkv", gkv_d[:, :], [128, 4])
                    csA, _, b_cs = sincos_tables(ph, "csb", posb[:, :], NBA, 1.0, want_b=False)
                    if stop == "L0":
                        em.barrier()
                        return nc
                    wlat = T(ph, "wlat", [128, 16, 576], BF16)
                    b_wlat = em.buf("wlat")
                    with ExitStack() as tmp:
                        wst = T(tmp, "Lwst", [128, 16, 288], F32)
                        b_wst = em.buf("Lwst")
                        for hh in range(2):
                            c0 = 512 + hh * 288
                            em.dma("sp", lambda e: e.dma_start(out=wst[:], in_=w_in[:, c0:c0 + 288].rearrange("(kc p) n -> p kc n", p=128)),
                                   writes=[b_wst])
                            em.op("dve", lambda e: e.tensor_tensor(out=wlat[:, :, hh * 288:(hh + 1) * 288], in0=wst[:],
                                                                   in1=g1[:].unsqueeze(2).to_broadcast([128, 16, 288]), op=ALU.mult), reads=[b_wst, b_g1],
                                  writes=[b_wlat], acc=(hh > 0))
                        em.barrier()
                    if stop == "L1":
                        em.barrier()
                        return nc
                    xt = [T(ph, f"Lx{i}", [128, 2048], F32) for i in range(3)]
                    b_xt = em.bufs("Lx", 3)
                    xs = [T(ph, f"Lxs{i}", [128, 2048], BF16) for i in range(3)]
                    b_xs = em.bufs("Lxs", 3)
                    hTb = [T(ph, f"LhT{i}", [128, 16, 128], BF16) for i in range(2)]
                    b_hTb = em.bufs("LhT", 2)
                    junk = T(ph, "Ljunk", [128, 2048], BF16)
                    b_junk = em.buf("Ljunk")
                    junk2 = T(ph, "Ljunk2", [128, 512], BF16)
                    b_junk2 = em.buf("Ljunk2")
                    stt = [T(ph, f"Lst{i}", [128, 8], F32) for i in range(5)]
                    b_stt = em.bufs("Lst", 5)
                    st2 = [T(ph, f"Lsu{i}", [128, 8], F32) for i in range(2)]
                    b_st2 = em.bufs("Lsu", 2)
                    ckvn = [T(ph, f"Lckvn{i}", [128, 512], BF16) for i in range(2)]
                    b_ckvn = em.bufs("Lckvn", 2)
                    ra = T(ph, "Lra", [128, 64], F32)
                    rb = T(ph, "Lrb", [128, 64], F32)
                    b_rab = em.buf("Lrab")
                    kr = [T(ph, f"Lkr{i}", [128, 128], BF16) for i in range(2)]
                    b_kr = em.bufs("Lkr", 2)
                    tpp = P(ph, "Ltp", [128, 16, 128], BF16)
                    b_tpp = em.buf("Ltp")
                    tp2s = [P(ph, f"Ltp2_{i}", [128, 8, 128], BF16) for i in range(2)]
                    b_tp2s = em.bufs("Ltp2_", 2)
                    plat = [P(ph, f"Lplat{i}", [128, 512], F32) for i in range(2)]
                    b_plat = em.bufs("Lplat", 2)
                    plat2 = [P(ph, f"Lplatb{i}", [128, 512], F32)[:, 0:64] for i in range(2)]
                    b_plat2 = em.bufs("Lplatb", 2)
                    NBL = NBA

                    def L_S1(blk):
                        j3 = blk % 3
                        j5 = blk % 5
                        em.dma("sp", lambda e: e.dma_start(out=xt[j3][:], in_=xb[blk * 128:(blk + 1) * 128, :]), writes=[b_xt[j3]])
                        em.op("act", lambda e: e.activation(out=junk[:], in_=xt[j3][:], func=AF.Square, accum_out=stt[j5][:, 0:1]),
                              reads=[b_xt[j3]], writes=[b_junk, b_stt[j5]])
                        em.op("act", lambda e: e.activation(out=xs[j3][:], in_=xt[j3][:], func=AF.Copy), reads=[b_xt[j3]], writes=[b_xs[j3]])
                        rstd_chain(stt[j5], b_stt[j5], 0, 1, 2048)

                    def L_S2(blk):
                        j3 = blk % 3
                        i = blk % 2
                        for kc in range(16):
                            em.op("pe", lambda e: e.transpose(out=tpp[:, kc, :], in_=xs[j3][:, kc * 128:(kc + 1) * 128], identity=identb[:]),
                                  reads=[b_xs[j3], b_identb], writes=[b_tpp], acc=(kc > 0))
                        em.op("act", lambda e: e.activation(out=hTb[i][:], in_=tpp[:], func=AF.Copy), reads=[b_tpp], writes=[b_hTb[i]])

                    def L_S3(blk):
                        i = blk % 2
                        for kc in range(16):
                            em.op("pe", lambda e: e.matmul(plat[i][:], lhsT=hTb[i][:, kc, :], rhs=wlat[:, kc, 0:512], start=(kc == 0),
                                                           stop=(kc == 15)), reads=[b_hTb[i], b_wlat], writes=[b_plat[i]], acc=(kc > 0))
                        for kc in range(16):
                            em.op("pe", lambda e: e.matmul(plat2[i], lhsT=hTb[i][:, kc, :], rhs=wlat[:, kc, 512:576], start=(kc == 0),
                                                           stop=(kc == 15)), reads=[b_hTb[i], b_wlat], writes=[b_plat2[i]], acc=(kc > 0))
                        j5 = blk % 5
                        em.op("act", lambda e: e.activation(out=junk2[:], in_=plat[i][:], func=AF.Square, accum_out=st2[i][:, 4:5]),
                              reads=[b_plat[i]], writes=[b_junk2, b_st2[i]])
                        em.op("dve", lambda e: e.tensor_scalar(out=st2[i][:, 3:4], in0=st2[i][:, 4:5], scalar1=1.0 / 512, scalar2=None, op0=ALU.mult),
                              reads=[b_st2[i]], writes=[b_st2[i]])
                        em.op("dve", lambda e: e.scalar_tensor_tensor(out=st2[i][:, 5:6], in0=stt[j5][:, 1:2], scalar=EPS, in1=st2[i][:, 3:4],
                                                                       op0=ALU.mult, op1=ALU.add), reads=[b_stt[j5], b_st2[i]], writes=[b_st2[i]])
                        em.op("act", lambda e: e.activation(out=st2[i][:, 6:7], in_=st2[i][:, 5:6], func=AF.Sqrt), reads=[b_st2[i]], writes=[b_st2[i]])
                        em.op("dve", lambda e: e.reciprocal(out=st2[i][:, 7:8], in_=st2[i][:, 6:7]), reads=[b_st2[i]], writes=[b_st2[i]])

                    def L_S3b(blk):
                        i = blk % 2
                        cr2 = 7
                        tp2 = tp2s[i]
                        b_tp2 = b_tp2s[i]
                        em.op("dve", lambda e: e.tensor_scalar(out=ckvn[i][:], in0=plat[i][:], scalar1=st2[i][:, cr2:cr2 + 1], scalar2=None, op0=ALU.mult),
                              reads=[b_plat[i], b_st2[i]], writes=[b_ckvn[i]])
                        for rc in range(4):
                            em.op("pe", lambda e: e.transpose(out=tp2[:, rc, :], in_=ckvn[i][:, rc * 128:(rc + 1) * 128], identity=identb[:]),
                                  reads=[b_ckvn[i], b_identb], writes=[b_tp2], acc=(rc > 0))
                        j5 = blk % 5
                        rr = stt[j5][:, 3:4]
                        em.op("dve", lambda e: e.scalar_tensor_tensor(out=ra[:], in0=plat2[i], scalar=rr, in1=csA[:, blk, :], op0=ALU.mult, op1=ALU.mult),
                              reads=[b_plat2[i], b_cs, b_stt[j5]], writes=[b_rab])
                        em.op("dve", lambda e: e.scalar_tensor_tensor(out=rb[:, 0:32], in0=plat2[i][:, 0:32], scalar=rr, in1=csA[:, blk, 32:64],
                                                                       op0=ALU.mult, op1=ALU.mult), reads=[b_plat2[i], b_cs, b_stt[j5]], writes=[b_rab], acc=True)
                        em.op("dve", lambda e: e.scalar_tensor_tensor(out=rb[:, 32:64], in0=plat2[i][:, 32:64], scalar=rr, in1=csA[:, blk, 0:32],
                                                                       op0=ALU.mult, op1=ALU.mult), reads=[b_plat2[i], b_cs, b_stt[j5]], writes=[b_rab], acc=True)
                        em.op("dve", lambda e: e.tensor_tensor(out=kr[i][:, 0:32], in0=ra[:, 0:32], in1=ra[:, 32:64], op=ALU.subtract),
                              reads=[b_rab], writes=[b_kr[i]])
                        em.op("dve", lambda e: e.tensor_tensor(out=kr[i][:, 32:64], in0=rb[:, 0:32], in1=rb[:, 32:64], op=ALU.add),
                              reads=[b_rab], writes=[b_kr[i]], acc=True)
                        em.op("dve", lambda e: e.tensor_copy(out=kr[i][:, 64:128], in_=kr[i][:, 0:64]), reads=[b_kr[i]], writes=[b_kr[i]],
                              acc=True)
                        em.op("pe", lambda e: e.transpose(out=tp2[:, 4, :], in_=kr[i][:], identity=identb[:]),
                              reads=[b_kr[i], b_identb], writes=[b_tp2], acc=True)

                    def L_S4(blk):
                        tp2 = tp2s[blk % 2]
                        b_tp2 = b_tp2s[blk % 2]
                        em.op("dve", lambda e: e.tensor_copy(out=ckvnT[:, blk, :, :], in_=tp2[:, 0:4, :]),
                              reads=[b_tp2], writes=[b_ckvnT[blk]])
                        em.op("dve", lambda e: e.tensor_copy(out=kpeT[:, blk * 128:(blk + 1) * 128], in_=tp2[:, 4, :]),
                              reads=[b_tp2], writes=[b_kpeT[blk]])

                    for k in range(NBL + 4):
                        if k < NBL:
                            L_S1(k)
                        if 0 <= k - 1 < NBL:
                            L_S2(k - 1)
                        if 0 <= k - 2 < NBL:
                            L_S3(k - 2)
                        if 0 <= k - 3 < NBL:
                            L_S3b(k - 3)
                        if 0 <= k - 4 < NBL:
                            L_S4(k - 4)
                    em.barrier()
                if stop == "L6":
                    em.barrier()
                    return nc
                if dbg:
                    for rc in range(4):
                        for hh in range(4):
                            em.dma("sp", lambda e: e.dma_start(out=d_ckvnT[:, rc, hh * 2048:(hh + 1) * 2048].rearrange("p (b t) -> p b t", t=128), in_=ckvnT[:, hh * 16:(hh + 1) * 16, rc, :]),
                                   reads=list(b_ckvnT))
                    em.dma("sp", lambda e: e.dma_start(out=d_kpeT[:, :], in_=kpeT[:]), reads=list(b_kpeT))
                if stop == "L":
                    em.barrier()
                    return nc

                with ExitStack() as ph:
                    gkvB, b_gkvB = load_small(ph, "Bgkv", gkv_d[:, :], [128, 4])
                    cmf, b_cmf = load_small(ph, "cmf", cmask_d[:, :], [128, 512])
                    cm = T(ph, "cm", [128, 512], BF16)
                    b_cm = em.buf("cm")
                    em.op("dve", lambda e: e.tensor_copy(out=cm[:], in_=cmf[:]), reads=[b_cmf], writes=[b_cm])
                    wq_st = [T(ph, f"Bwqst{i}", [128, 4, 128], F32) for i in range(2)]
                    wkv_st = [T(ph, f"Bwkvst{i}", [128, 4, 256], F32) for i in range(2)]
                    b_wst = em.bufs("Bwst", 2)
                    wq_bf = [T(ph, f"Bwq{i}", [128, 4, 128], BF16) for i in range(2)]
                    wkv_bf = [T(ph, f"Bwkv{i}", [128, 4, 256], BF16) for i in range(2)]
                    b_wbf = em.bufs("Bwbf", 2)
                    KnT = T(ph, "KnT", [128, NBA * 128], BF16)
                    b_KnT = em.bufs("KnT", 16)
                    Vh = T(ph, "Vh", [128, NBA, 132], BF16)
                    b_Vh = em.bufs("Vh", 16)
                    b_Vones = em.buf("Vones")
                    qnT = T(ph, "qnT", [128, NOWN * 128], BF16)
                    b_qnT = em.bufs("qnT", 4)
                    qrTh = [T(ph, f"qrTh{i}", [128, NOWN * 128], BF16) for i in range(2)]
                    b_qrTh = em.bufs("qrTh", 2)
                    pT = [T(ph, f"pT{i}", [128, 512], BF16) for i in range(3)]
                    b_pT = em.bufs("pT", 3)
                    oah = [T(ph, f"oah{i}", [128, NOWN, 128], BF16) for i in range(2)]
                    b_oah = em.bufs("oah", 2)
                    rs = [T(ph, f"Brs{i}", [128, 1], F32) for i in range(2)]
                    b_rs = em.bufs("Brs", 2)
                    pbld = [P(ph, f"Bpb{i}", [128, 512], F32) for i in range(2)]
                    b_pbld = em.bufs("Bpb", 2)
                    pS = [P(ph, f"BpS{i}", [128, 512], F32) for i in range(2)]
                    b_pS = em.bufs("BpS", 2)
                    pO = [P(ph, f"BpO{i}", [128, 512], F32) for i in range(2)]
                    b_pO = em.bufs("BpO", 2)

                    em.op("pool", lambda e: e.memset(Vh[:, :, 128:132], 1.0), writes=[b_Vones])
                    w_uq_v = w_uq.rearrange("(rc p) c -> p rc c", p=128)
                    w_ukv_v = w_ukv.rearrange("(rc p) c -> p rc c", p=128)

                    def load_head_w(h):
                        i = h % 2
                        em.dma("sp", lambda e: e.dma_start(out=wq_st[i][:], in_=w_uq_v[:, :, h * 192:h * 192 + 128]), writes=[b_wst[i]])
                        em.dma("sp", lambda e: e.dma_start(out=wkv_st[i][:], in_=w_ukv_v[:, :, h * 256:(h + 1) * 256]), writes=[b_wst[i]],
                               group=True)
                        po = (h % 2) * 64
                        em.dma("sp", lambda e: e.dma_start(out=qrTh[i][po:po + 64, :], in_=qrs[po:po + 64, h // 2, :]), writes=[b_qrTh[i]])

                    load_head_w(0)
                    nb = 0
                    ev_i = 0
                    for h in range(NH):
                        i = h % 2
                        po = (h % 2) * 64
                        if h + 1 < NH:
                            load_head_w(h + 1)
                        emit_tab_jobs(4)
                        em.op("dve", lambda e: e.tensor_copy(out=wq_bf[i][:], in_=wq_st[i][:]), reads=[b_wst[i]], writes=[b_wbf[i]])
                        for rc in range(4):
                            em.op("dve", lambda e: e.tensor_scalar(out=wkv_bf[i][:, rc, :], in0=wkv_st[i][:, rc, :], scalar1=gkvB[:, rc:rc + 1],
                                                                   scalar2=None, op0=ALU.mult), reads=[b_wst[i], b_gkvB], writes=[b_wbf[i]], acc=True)
                        for qc in range(4):
                            pb = pbld[nb % 2]
                            b_pb = b_pbld[nb % 2]
                            nb += 1
                            for rc in range(4):
                                em.op("pe", lambda e: e.matmul(pb[:], lhsT=wq_bf[i][:, rc, :], rhs=cqnT[:, rc, qc * 512:(qc + 1) * 512],
                                                               start=(rc == 0), stop=(rc == 3)),
                                      reads=[b_wbf[i]] + b_cqnT[qc * 4:(qc + 1) * 4], writes=[b_pb], acc=(rc > 0))
                            em.op("act", lambda e: e.activation(out=qnT[:, qc * 512:(qc + 1) * 512], in_=pb[:], func=AF.Copy, scale=QSCALE),
                                  reads=[b_pb], writes=[b_qnT[qc]])
                        for tc in range(16):
                            pb = pbld[nb % 2]
                            b_pb = b_pbld[nb % 2]
                            nb += 1
                            for rc in range(4):
                                em.op("pe", lambda e: e.matmul(pb[:], lhsT=wkv_bf[i][:, rc, 0:128], rhs=ckvnT[:, tc * 4:(tc + 1) * 4, rc, :],
                                                               start=(rc == 0), stop=(rc == 3)),
                                      reads=[b_wbf[i]] + b_ckvnT[tc * 4:(tc + 1) * 4], writes=[b_pb], acc=(rc > 0))
                            eng = "act" if (ev_i % 2 == 0) else "dve"
                            ev_i += 1
                            if eng == "act":
                                em.op("act", lambda e: e.activation(out=KnT[:, tc * 512:(tc + 1) * 512], in_=pb[:], func=AF.Copy),
                                      reads=[b_pb], writes=[b_KnT[tc]])
                            else:
                                em.op("dve", lambda e: e.tensor_copy(out=KnT[:, tc * 512:(tc + 1) * 512], in_=pb[:]),
                                      reads=[b_pb], writes=[b_KnT[tc]])
                        for g in range(16):
                            pb = pbld[nb % 2]
                            b_pb = b_pbld[nb % 2]
                            nb += 1
                            for ii in range(4):
                                blk = 4 * g + ii
                                for rc in range(4):
                                    em.op("pe", lambda e: e.matmul(pb[:, ii * 128:(ii + 1) * 128], lhsT=ckvnT[:, blk, rc, :],
                                                                   rhs=wkv_bf[i][:, rc, 128:256], start=(rc == 0), stop=(rc == 3)),
                                          reads=[b_wbf[i], b_ckvnT[blk]], writes=[b_pb], acc=not (ii == 0 and rc == 0))
                            eng = "act" if (ev_i % 2 == 0) else "dve"
                            ev_i += 1
                            src = pb[:].rearrange("p (a d) -> p a d", d=128)
                            if eng == "act":
                                em.op("act", lambda e: e.activation(out=Vh[:, 4 * g:4 * g + 4, 0:128], in_=src, func=AF.Copy),
                                      reads=[b_pb], writes=[b_Vh[g]])
                            else:
                                em.op("dve", lambda e: e.tensor_copy(out=Vh[:, 4 * g:4 * g + 4, 0:128], in_=src),
                                      reads=[b_pb], writes=[b_Vh[g]])

                        work = [(m, c) for m in range(NOWN) for c in range(m + 1)]

                        def emit_S(k):
                            m, c = work[k]
                            ps = pS[k % 2]
                            b_ps = b_pS[k % 2]
                            for ii in range(4):
                                kb = 4 * c + ii
                                em.op("pe", lambda e: e.matmul(ps[:, ii * 128:(ii + 1) * 128], lhsT=KnT[:, kb * 128:(kb + 1) * 128],
                                                               rhs=qnT[:, m * 128:(m + 1) * 128], start=True, stop=False),
                                      reads=[b_KnT[c], b_qnT[m // 4]], writes=[b_ps], acc=(ii > 0))
                                em.op("pe", lambda e: e.matmul(ps[:, ii * 128:(ii + 1) * 128], lhsT=kpeT[po:po + 64, kb * 128:(kb + 1) * 128],
                                                               rhs=qrTh[i][po:po + 64, m * 128:(m + 1) * 128], start=False, stop=True),
                                      reads=[b_kpeT[kb], b_qrTh[i]], writes=[b_ps], acc=True)
                            p = pT[k % 3]
                            b_p = b_pT[k % 3]
                            em.op("act", lambda e: e.activation(out=p[:], in_=ps[:], func=AF.Exp), reads=[b_ps], writes=[b_p])
                            if c == m:
                                em.op("dve", lambda e: e.tensor_tensor(out=p[:], in0=p[:], in1=cm[:], op=ALU.mult), reads=[b_p, b_cm],
                                      writes=[b_p])

                        def emit_PV(k):
                            m, c = work[k]
                            p = pT[k % 3]
                            b_p = b_pT[k % 3]
                            po_ = pO[m % 2]
                            b_po = b_pO[m % 2]
                            for ii in range(4):
                                kb = 4 * c + ii
                                em.op("pe", lambda e: e.matmul(po_[:, 0:129], lhsT=p[:, ii * 128:(ii + 1) * 128], rhs=Vh[:, kb, 0:129],
                                                               start=(c == 0 and ii == 0), stop=(c == m and ii == 3)),
                                      reads=[b_p, b_Vh[c], b_Vones], writes=[b_po], acc=not (c == 0 and ii == 0))
                            if c == m:
                                r = rs[m % 2]
                                b_r = b_rs[m % 2]
                                em.op("dve", lambda e: e.reciprocal(out=r[:], in_=po_[:, 128:129]), reads=[b_po], writes=[b_r])
                                em.op("dve", lambda e: e.tensor_scalar(out=oah[i][:, m, :], in0=po_[:, 0:128], scalar1=r[:, 0:1], scalar2=None,
                                                                       op0=ALU.mult), reads=[b_po, b_r], writes=[b_oah[i]], acc=(m > 0))

                        emit_S(0)
                        for k in range(len(work)):
                            if k + 1 < len(work):
                                emit_S(k + 1)
                            emit_PV(k)
                        em.dma("pool", lambda e: e.dma_start(out=oas[:, h * 128:(h + 1) * 128].rearrange("(m p) d -> p m d", p=128),
                                                             in_=oah[i][:]), reads=[b_oah[i]])
                    em.barrier()
        if stop == "B":
            em.barrier()
            return nc

        with ExitStack() as ph:
            lng, b_lng = load_small(ph, "lng", lng_d[:, :], [128, 2048])
            lnb, b_lnb = load_small(ph, "lnb", lnb_d[:, :], [128, 2048])
            bsp, b_bsp = load_small(ph, "bsp", bsp_d[:, :], [128, 8])
            wspf, b_wspf = load_small(ph, "wspf", wspT_d[:, :, :], [128, 8, 128])
            trif, b_trif = load_small(ph, "trif", triT_d[:, :], [128, 128])
            wsT = T(ph, "wsT", [128, 8, 128], BF16)
            b_wsT = em.buf("wsT")
            em.op("dve", lambda e: e.tensor_tensor(out=wsT[:], in0=wspf[:], in1=trif[:].unsqueeze(1).to_broadcast([128, 8, 128]),
                                                   op=ALU.mult), reads=[b_wspf, b_trif], writes=[b_wsT])
            wo = T(ph, "wo", [128, 16, 2048], BF16)
            b_wo = em.bufs("wo", 4)
            with ExitStack() as tmp:
                wst = T(tmp, "Cwst", [128, 16, 256], F32)
                b_wst = em.buf("Cwst")
                for dc8 in range(8):
                    em.dma("sp", lambda e: e.dma_start(out=wst[:], in_=w_out[:, dc8 * 256:(dc8 + 1) * 256].rearrange("(kc p) n -> p kc n", p=128)),
                           writes=[b_wst])
                    for hh in range(2):
                        if hh == 0:
                            em.op("dve", lambda e: e.tensor_copy(out=wo[:, hh * 8:(hh + 1) * 8, dc8 * 256:(dc8 + 1) * 256],
                                                                 in_=wst[:, hh * 8:(hh + 1) * 8, :]), reads=[b_wst], writes=[b_wo[dc8 // 2]], acc=True)
                        else:
                            em.op("act", lambda e: e.activation(out=wo[:, hh * 8:(hh + 1) * 8, dc8 * 256:(dc8 + 1) * 256],
                                                                in_=wst[:, hh * 8:(hh + 1) * 8, :], func=AF.Copy), reads=[b_wst], writes=[b_wo[dc8 // 2]], acc=True)
                em.barrier()
            ld = {}
            for nm in ["u", "v", "gA", "gB", "oa"]:
                ld[nm] = ([T(ph, f"C{nm}{i}", [128, 2048], BF16) for i in range(2)], em.bufs(f"C{nm}", 2))
            xt = [T(ph, f"Cx{i}", [128, 2048], F32) for i in range(2)]
            b_xt = em.bufs("Cx", 2)
            junk = T(ph, "Cjunk", [128, 2048], BF16)
            b_junk = em.buf("Cjunk")
            stt = T(ph, "Cst", [128, 16], F32)
            b_stt = em.buf("Cst")
            tf = T(ph, "Ctf", [128, 2048], F32)
            b_tf = em.buf("Ctf")
            vn = T(ph, "Cvn", [128, 2048], BF16)
            b_vn = em.buf("Cvn")
            ob = tf
            b_ob = b_tf
            t1 = T(ph, "Ct1", [128, 2048], BF16)
            b_t1 = em.buf("Ct1")
            mg = T(ph, "Cmg", [128, 2048], BF16)
            b_mg = em.buf("Cmg")
            mT = T(ph, "CmT", [128, 16, 128], BF16)
            b_mT = em.buf("CmT")
            x1 = [T(ph, "Cxone", [128, 2048], F32)] * 2
            b_x1 = [em.buf("Cxone")] * 2
            pbig = P(ph, "Cpbig", [128, 2048], F32)
            b_pbig = em.buf("Cpbig")
            tpp = P(ph, "Ctp", [128, 16, 128], BF16)
            b_tpp = em.buf("Ctp")

            pmix = P(ph, "Cpmix", [128, 1024], F32)
            b_pmix = em.buf("Cpmix")
            mgs = [mg, T(ph, "Cmg1", [128, 2048], BF16)]
            b_mgs = [b_mg, em.buf("Cmg1")]

            def c1_loads(m):
                i = m % 2
                r0 = m * 128
                em.dma("sp", lambda e: e.dma_start(out=ld["u"][0][i][:], in_=zs[r0:r0 + 128, 0:2048]), writes=[ld["u"][1][i]])
                em.dma("sp", lambda e: e.dma_start(out=ld["v"][0][i][:], in_=zs[r0:r0 + 128, 2048:4096]), writes=[ld["v"][1][i]])
                em.dma("sp", lambda e: e.dma_start(out=ld["gA"][0][i][:], in_=gs[r0:r0 + 128, 0:2048]), writes=[ld["gA"][1][i]])
                em.dma("sp", lambda e: e.dma_start(out=ld["gB"][0][i][:], in_=gs[r0:r0 + 128, 2048:4096]), writes=[ld["gB"][1][i]])
                em.dma("sp", lambda e: e.dma_start(out=ld["oa"][0][i][:], in_=oas[r0:r0 + 128, :]), writes=[ld["oa"][1][i]])

            def C1_S1(m):
                i = m % 2
                if m + 1 < NOWN:
                    c1_loads(m + 1)
                u, b_u = ld["u"][0][i], ld["u"][1][i]
                v, b_v = ld["v"][0][i], ld["v"][1][i]
                gA, b_gA = ld["gA"][0][i], ld["gA"][1][i]
                gB, b_gB = ld["gB"][0][i], ld["gB"][1][i]
                oa, b_oa = ld["oa"][0][i], ld["oa"][1][i]
                mgc, b_mgc = mgs[i], b_mgs[i]
                em.op("act", lambda e: e.activation(out=junk[:], in_=v[:], func=AF.Copy, accum_out=stt[:, 0:1]), reads=[b_v], writes=[b_junk, b_stt])
                em.op("act", lambda e: e.activation(out=junk[:], in_=v[:], func=AF.Square, accum_out=stt[:, 1:2]), reads=[b_v], writes=[b_junk, b_stt],
                      acc=True)
                em.op("dve", lambda e: e.tensor_scalar(out=stt[:, 2:4], in0=stt[:, 0:2], scalar1=1.0 / 2048, scalar2=None, op0=ALU.mult),
                      reads=[b_stt], writes=[b_stt])
                em.op("dve", lambda e: e.tensor_tensor(out=stt[:, 4:5], in0=stt[:, 2:3], in1=stt[:, 2:3], op=ALU.mult),
                      reads=[b_stt], writes=[b_stt])
                em.op("dve", lambda e: e.tensor_tensor(out=stt[:, 5:6], in0=stt[:, 3:4], in1=stt[:, 4:5], op=ALU.subtract),
                      reads=[b_stt], writes=[b_stt])
                em.op("dve", lambda e: e.tensor_scalar(out=stt[:, 6:7], in0=stt[:, 5:6], scalar1=EPS, scalar2=None, op0=ALU.add),
                      reads=[b_stt], writes=[b_stt])
                em.op("act", lambda e: e.activation(out=stt[:, 7:8], in_=stt[:, 6:7], func=AF.Sqrt), reads=[b_stt], writes=[b_stt])
                em.op("dve", lambda e: e.reciprocal(out=stt[:, 8:9], in_=stt[:, 7:8]), reads=[b_stt], writes=[b_stt])
                em.op("dve", lambda e: e.tensor_scalar(out=tf[:], in0=v[:], scalar1=stt[:, 2:3], scalar2=stt[:, 8:9], op0=ALU.subtract,
                                                       op1=ALU.mult), reads=[b_v, b_stt], writes=[b_tf])
                em.op("dve", lambda e: e.tensor_tensor(out=tf[:], in0=tf[:], in1=lng[:], op=ALU.mult), reads=[b_tf, b_lng], writes=[b_tf])
                em.op("dve", lambda e: e.tensor_tensor(out=vn[:], in0=tf[:], in1=lnb[:], op=ALU.add), reads=[b_tf, b_lnb], writes=[b_vn])

            def C1_S1b(m):
                i = m % 2
                u, b_u = ld["u"][0][i], ld["u"][1][i]
                gA, b_gA = ld["gA"][0][i], ld["gA"][1][i]
                gB, b_gB = ld["gB"][0][i], ld["gB"][1][i]
                oa, b_oa = ld["oa"][0][i], ld["oa"][1][i]
                mgc, b_mgc = mgs[i], b_mgs[i]
                for half in range(2):
                    for gg in range(4):
                        g = half * 4 + gg
                        em.op("pe", lambda e: e.matmul(pmix[:, gg * 256:(gg + 1) * 256], lhsT=wsT[:, g, :], rhs=vn[:, g * 256:(g + 1) * 256],
                                                       start=True, stop=True), reads=[b_wsT, b_vn], writes=[b_pmix], acc=(gg > 0))
                    for gg in range(4):
                        g = half * 4 + gg
                        em.op("dve", lambda e: e.scalar_tensor_tensor(out=ob[:, g * 256:(g + 1) * 256], in0=pmix[:, gg * 256:(gg + 1) * 256],
                                                                       scalar=bsp[:, g:g + 1], in1=u[:, g * 256:(g + 1) * 256], op0=ALU.add,
                                                                       op1=ALU.mult), reads=[b_pmix, b_bsp, b_u], writes=[b_ob], acc=(g > 0))
                em.op("dve", lambda e: e.tensor_tensor(out=t1[:], in0=gA[:], in1=oa[:], op=ALU.mult), reads=[b_gA, b_oa], writes=[b_t1])
                em.op("dve", lambda e: e.tensor_tensor(out=ob[:], in0=ob[:], in1=gB[:], op=ALU.mult), reads=[b_ob, b_gB], writes=[b_ob])
                em.op("dve", lambda e: e.tensor_tensor(out=mgc[:], in0=ob[:], in1=t1[:], op=ALU.add), reads=[b_ob, b_t1], writes=[b_mgc])

            def C1_S2(m):
                i = m % 2
                mgc, b_mgc = mgs[i], b_mgs[i]
                em.dma("sp", lambda e: e.dma_start(out=xt[i][:], in_=xo[m * 128:(m + 1) * 128, :]), writes=[b_xt[i]])
                for kc in range(16):
                    em.op("pe", lambda e: e.transpose(out=tpp[:, kc, :], in_=mgc[:, kc * 128:(kc + 1) * 128], identity=identb[:]),
                          reads=[b_mgc, b_identb], writes=[b_tpp], acc=(kc > 0))
                em.op("act", lambda e: e.activation(out=mT[:], in_=tpp[:], func=AF.Copy), reads=[b_tpp], writes=[b_mT])
                for dc in range(4):
                    for kc in range(16):
                        em.op("pe", lambda e: e.matmul(pbig[:, dc * 512:(dc + 1) * 512], lhsT=mT[:, kc, :], rhs=wo[:, kc, dc * 512:(dc + 1) * 512],
                                                       start=(kc == 0), stop=(kc == 15)), reads=[b_mT, b_wo[dc]], writes=[b_pbig],
                              acc=not (dc == 0 and kc == 0))
                em.op("dve", lambda e: e.tensor_tensor(out=x1[i][:], in0=pbig[:], in1=xt[i][:], op=ALU.add), reads=[b_pbig, b_xt[i]],
                      writes=[b_x1[i]])
                em.dma("pool", lambda e: e.dma_start(out=x1s[m * 128:(m + 1) * 128, :], in_=x1[i][:]), reads=[b_x1[i]])

            c1_loads(0)
            C1_S1(0)
            C1_S1b(0)
            for m in range(NOWN):
                if m + 1 < NOWN:
                    C1_S1(m + 1)
                C1_S2(m)
                if m + 1 < NOWN:
                    C1_S1b(m + 1)
            em.barrier()
        if stop == "C1":
            em.barrier()
            return nc

        emit_tab_jobs(64)
        with ExitStack() as ph:
            g2b, b_g2b = load_small(ph, "g2b", g2_d[:, :], [128, 2048])
            gfb, b_gfb = load_small(ph, "gfb", gf_d[:, :], [128, 2048])
            iota16, b_iota4 = load_small(ph, "iota16", iota4_d[:, 0:16], [128, 16])
            kT = T(ph, "kT", [128, 16, 128], BF16)
            b_kT = em.buf("kT")
            wpq = T(ph, "wpq", [128, 16, 2048], BF16)
            b_wpq = em.bufs("wpq", 4)
            with ExitStack() as tmp:
                kTf, b_kTf = load_small(tmp, "kTf", keysT_d[:, :, :], [128, 16, 128])
                em.op("dve", lambda e: e.tensor_copy(out=kT[:], in_=kTf[:]), reads=[b_kTf], writes=[b_kT])
                wst = T(tmp, "Dwst", [128, 16, 512], F32)
                b_wst = em.buf("Dwst")
                for dc in range(4):
                    em.dma("sp", lambda e: e.dma_start(out=wst[:], in_=w_pq[:, dc * 512:(dc + 1) * 512].rearrange("(kc p) n -> p kc n", p=128)),
                           writes=[b_wst])
                    em.op("dve", lambda e: e.tensor_copy(out=wpq[:, :, dc * 512:(dc + 1) * 512], in_=wst[:]), reads=[b_wst], writes=[b_wpq[dc]])
                em.barrier()
            NGU = 8
            NGV = 4
            ug = [T(ph, f"ug{i}", [128, 2048], BF16) for i in range(NGU)]
            b_ug = em.bufs("ug", NGU)
            vg = [T(ph, f"vg{i}", [128, 2048], BF16) for i in range(NGV)]
            b_vg = em.bufs("vg", NGV)
            xP = T(ph, "DxP", [128, 2048], F32)
            b_xP = em.buf("DxP")
            xE = T(ph, "DxE", [128, 2048], F32)
            b_xE = em.buf("DxE")
            stt = T(ph, "Dst", [128, 16], F32)
            b_stt = em.buf("Dst")
            stE = T(ph, "DstE", [128, 16], F32)
            b_stE = em.buf("DstE")
            h2 = [T(ph, f"Dh2_{i}", [128, 2048], BF16) for i in range(2)]
            b_h2 = em.bufs("Dh2_", 2)
            h2T = T(ph, "Dh2T", [128, 16, 128], BF16)
            b_h2T = em.buf("Dh2T")
            qb = T(ph, "Dqb", [128, 2048], BF16)
            b_qb = em.buf("Dqb")
            qT = h2T
            b_qT = b_h2T
            scr = T(ph, "Dscr", [128, 2048], F32)
            b_scr = em.buf("Dscr")
            sc = scr[:].rearrange("p (a n) -> p a n", n=128)
            b_sc = b_scr
            cand = scr[:].rearrange("p (h c) -> p h c", c=256)
            b_cand = b_scr
            eq = scr
            b_eq = b_scr
            scrE = T(ph, "DscrE", [128, 1024], BF16)
            b_scrE = em.buf("DscrE")
            wk = T(ph, "Dwk", [128, 256], F32)
            b_wk = em.buf("Dwk")
            t16 = T(ph, "Dt16", [128, 16, 16], F32)
            b_t16 = em.buf("Dt16")
            i16 = T(ph, "Di16", [128, 16, 16], U32)
            b_i16 = em.buf("Di16")
            i16f = T(ph, "Di16f", [128, 16, 16], F32)
            b_i16f = em.buf("Di16f")
            ts = T(ph, "Dts", [128, 8, 16], F32)
            b_ts = em.buf("Dts")
            pidx = T(ph, "Dpidx", [128, 8, 16], U32)
            b_pidx = em.buf("Dpidx")
            pa_i = T(ph, "Dpai", [128, 8, 16], U32)
            pb_i = T(ph, "Dpbi", [128, 8, 16], U32)
            pa_f = T(ph, "Dpaf", [128, 8, 16], F32)
            pb_f = T(ph, "Dpbf", [128, 8, 16], F32)
            b_pab = em.buf("Dpab")
            isel = T(ph, "Disel", [128, 2, 128], F32)
            b_isel = em.buf("Disel")
            idxf = T(ph, "Didxf", [128, 128], F32)
            b_idxf = em.buf("Didxf")
            ex = T(ph, "Dex", [128, 8, 16], F32)
            b_ex = em.buf("Dex")
            zz = T(ph, "Dzz", [128, 16], F32)
            b_zz = em.buf("Dzz")
            gate = T(ph, "Dgate", [128, 128], F32)
            b_gate = em.buf("Dgate")
            idxT = [T(ph, f"DidxT{i}", [128, 128], I32) for i in range(3)]
            b_idxT = em.bufs("DidxT", 3)
            gateT = [T(ph, f"DgateT{i}", [128, 128], F32) for i in range(3)]
            b_gateT = em.bufs("DgateT", 3)
            hu4 = T(ph, "Dhu4", [128, 4, 128], F32)
            b_hu4 = em.buf("Dhu4")
            hu = T(ph, "Dhu", [128, 128], F32)
            b_hu = em.buf("Dhu")
            Aa = [T(ph, f"DA{i}", [128, 128], F32) for i in range(2)]
            b_Aa = em.bufs("DA", 2)
            AD = [T(ph, f"DAD{i}", [128, 32 * 128], BF16) for i in range(2)]
            b_AD = em.bufs("DAD", 2)
            pA = P(ph, "DpA", [128, 2048], F32)
            b_pA = em.buf("DpA")
            bq = [P(ph, f"Dbq{i}", [128, 1024], F32) for i in range(2)]
            b_bq = em.bufs("Dbq", 2)
            pP = [bq[0][:, 0:512], bq[1][:, 0:512]]
            b_pP = b_bq
            pPb = [pP[k].bitcast(BF16).rearrange("p (k t) -> p k t", t=128) for k in range(2)]

            em.op("dve", lambda e: e.memset(AD[0][:], 0.0), writes=[b_AD[0]])
            em.op("dve", lambda e: e.memset(AD[1][:], 0.0), writes=[b_AD[1]])
            cnt = {"pp": 0, "bq": 0, "gu": 0, "gv": 0}

            def next_pp():
                k = cnt["pp"] % 2
                cnt["pp"] += 1
                return k

            def stage_P_gen(m):
                xx = xP
                b_xx = b_xP
                hh2 = h2[m % 2]
                b_hh2 = b_h2[m % 2]
                em.dma("sp", lambda e: e.dma_start(out=xx[:], in_=x1s[m * 128:(m + 1) * 128, :]), writes=[b_xx])
                em.op("dve", lambda e: e.memset(stt[:], 0.0), writes=[b_stt])
                em.op("dve", lambda e: e.scalar_tensor_tensor(out=scr[:], in0=xx[:], scalar=1.0, in1=xx[:], op0=ALU.mult, op1=ALU.mult,
                                                               accum_out=stt[:, 0:1]), reads=[b_xx], writes=[b_scr, b_stt])
                cr = rstd_chain(stt, b_stt, 0, 1, 2048)
                em.op("dve", lambda e: e.scalar_tensor_tensor(out=hh2[:], in0=xx[:], scalar=stt[:, cr:cr + 1], in1=g2b[:], op0=ALU.mult,
                                                               op1=ALU.mult), reads=[b_xx, b_stt, b_g2b], writes=[b_hh2])
                for half in range(2):
                    yield
                    k = next_pp()
                    for kk in range(8):
                        kc = half * 8 + kk
                        em.op("pe", lambda e: e.transpose(out=pPb[k][:, kk, :], in_=hh2[:, kc * 128:(kc + 1) * 128], identity=identb[:]),
                              reads=[b_hh2, b_identb], writes=[b_pP[k]], acc=(kk > 0))
                    em.op("act", lambda e: e.activation(out=h2T[:, half * 8:(half + 1) * 8, :], in_=pPb[k], func=AF.Copy),
                          reads=[b_pP[k]], writes=[b_h2T], acc=(half > 0))
                for dc in range(4):
                    yield
                    k = next_pp()
                    for kc in range(16):
                        em.op("pe", lambda e: e.matmul(pP[k][:], lhsT=h2T[:, kc, :], rhs=wpq[:, kc, dc * 512:(dc + 1) * 512],
                                                       start=(kc == 0), stop=(kc == 15)), reads=[b_h2T, b_wpq[dc]], writes=[b_pP[k]],
                              acc=(kc > 0))
                    em.op("act", lambda e: e.activation(out=qb[:, dc * 512:(dc + 1) * 512], in_=pP[k][:], func=AF.Copy),
                          reads=[b_pP[k]], writes=[b_qb], acc=(dc > 0))
                for half in range(2):
                    yield
                    k = next_pp()
                    for kk in range(8):
                        hp = half * 8 + kk
                        em.op("pe", lambda e: e.transpose(out=pPb[k][:, kk, :], in_=qb[:, hp * 128:(hp + 1) * 128], identity=identb[:]),
                              reads=[b_qb, b_identb], writes=[b_pP[k]], acc=(kk > 0))
                    em.op("act", lambda e: e.activation(out=qT[:, half * 8:(half + 1) * 8, :], in_=pPb[k], func=AF.Copy),
                          reads=[b_pP[k]], writes=[b_qT], acc=(half > 0))
                for qd in range(4):
                    yield
                    k = next_pp()
                    for ii in range(4):
                        hp = qd * 4 + ii
                        em.op("pe", lambda e: e.matmul(pP[k][:, ii * 128:(ii + 1) * 128], lhsT=qT[:, hp, :], rhs=kT[:, hp, :], start=True, stop=True),
                              reads=[b_qT, b_kT], writes=[b_pP[k]], acc=(ii > 0))
                    em.op("act", lambda e: e.activation(out=scr[:, qd * 512:(qd + 1) * 512], in_=pP[k][:], func=AF.Copy),
                          reads=[b_pP[k]], writes=[b_sc], acc=(qd > 0))
                for hp in range(16):
                    yield
                    em.op("dve", lambda e: e.max(out=t16[:, hp, 0:8], in_=sc[:, hp, :]), reads=[b_sc], writes=[b_t16], acc=True)
                    em.op("dve", lambda e: e.max_index(out=i16[:, hp, 0:8], in_max=t16[:, hp, 0:8], in_values=sc[:, hp, :]),
                          reads=[b_sc, b_t16], writes=[b_i16], acc=True)
                    em.op("dve", lambda e: e.match_replace(out=wk[:, 0:128], in_to_replace=t16[:, hp, 0:8], in_values=sc[:, hp, :],
                                                           imm_value=-1e30), reads=[b_sc, b_t16], writes=[b_wk])
                    em.op("dve", lambda e: e.max(out=t16[:, hp, 8:16], in_=wk[:, 0:128]), reads=[b_wk], writes=[b_t16], acc=True)
                    em.op("dve", lambda e: e.max_index(out=i16[:, hp, 8:16], in_max=t16[:, hp, 8:16], in_values=wk[:, 0:128]),
                          reads=[b_wk, b_t16], writes=[b_i16], acc=True)
                yield
                em.op("dve", lambda e: e.tensor_copy(out=i16f[:], in_=i16[:]), reads=[b_i16], writes=[b_i16f])
                t16v = t16[:].rearrange("p (h two) k -> p h two k", two=2)
                i16v = i16f[:].rearrange("p (h two) k -> p h two k", two=2)
                cand4 = cand.rearrange("p h (a b) -> p h a b", b=16)
                em.op("dve", lambda e: e.tensor_tensor(out=cand4, in0=t16v[:, :, 0, :].unsqueeze(3).to_broadcast([128, 8, 16, 16]),
                                                       in1=t16v[:, :, 1, :].unsqueeze(2).to_broadcast([128, 8, 16, 16]), op=ALU.add),
                      reads=[b_t16], writes=[b_cand])
                for hh in range(8):
                    yield
                    em.op("dve", lambda e: e.max(out=ts[:, hh, 0:8], in_=cand[:, hh, :]), reads=[b_cand], writes=[b_ts], acc=True)
                    em.op("dve", lambda e: e.max_index(out=pidx[:, hh, 0:8], in_max=ts[:, hh, 0:8], in_values=cand[:, hh, :]),
                          reads=[b_cand, b_ts], writes=[b_pidx], acc=True)
                    em.op("dve", lambda e: e.match_replace(out=wk[:], in_to_replace=ts[:, hh, 0:8], in_values=cand[:, hh, :],
                                                           imm_value=-1e30), reads=[b_cand, b_ts], writes=[b_wk])
                    em.op("dve", lambda e: e.max(out=ts[:, hh, 8:16], in_=wk[:]), reads=[b_wk], writes=[b_ts], acc=True)
                    em.op("dve", lambda e: e.max_index(out=pidx[:, hh, 8:16], in_max=ts[:, hh, 8:16], in_values=wk[:]),
                          reads=[b_wk, b_ts], writes=[b_pidx], acc=True)
                yield
                em.op("dve", lambda e: e.tensor_tensor(out=ex[:], in0=ts[:], in1=ts[:, :, 0:1].to_broadcast([128, 8, 16]), op=ALU.subtract),
                      reads=[b_ts], writes=[b_ex])
                em.op("act", lambda e: e.activation(out=ex[:], in_=ex[:], func=AF.Exp), reads=[b_ex], writes=[b_ex])
                em.op("dve", lambda e: e.tensor_reduce(out=zz[:, 0:8], in_=ex[:], axis=AX.X, op=ALU.add), reads=[b_ex], writes=[b_zz])
                em.op("dve", lambda e: e.reciprocal(out=zz[:, 8:16], in_=zz[:, 0:8]), reads=[b_zz], writes=[b_zz])
                em.op("dve", lambda e: e.tensor_tensor(out=gate[:].rearrange("p (h k) -> p h k", k=16), in0=ex[:],
                                                       in1=zz[:, 8:16].unsqueeze(2).to_broadcast([128, 8, 16]), op=ALU.mult),
                      reads=[b_ex, b_zz], writes=[b_gate])
                em.op("dve", lambda e: e.tensor_scalar(out=pa_i[:], in0=pidx[:], scalar1=4, scalar2=None, op0=ALU.logical_shift_right),
                      reads=[b_pidx], writes=[b_pab])
                em.op("dve", lambda e: e.tensor_scalar(out=pb_i[:], in0=pidx[:], scalar1=15, scalar2=None, op0=ALU.bitwise_and),
                      reads=[b_pidx], writes=[b_pab], acc=True)
                em.op("dve", lambda e: e.tensor_copy(out=pa_f[:], in_=pa_i[:]), reads=[b_pab], writes=[b_pab], acc=True)
                em.op("dve", lambda e: e.tensor_copy(out=pb_f[:], in_=pb_i[:]), reads=[b_pab], writes=[b_pab], acc=True)
                eq4 = eq[:].rearrange("p (h k a) -> p h k a", h=8, k=16)
                io4 = iota16[:].unsqueeze(1).unsqueeze(1).to_broadcast([128, 8, 16, 16])
                for half, pf in enumerate([pa_f, pb_f]):
                    yield
                    em.op("dve", lambda e: e.tensor_tensor(out=eq4, in0=pf[:].unsqueeze(3).to_broadcast([128, 8, 16, 16]), in1=io4,
                                                           op=ALU.is_equal), reads=[b_pab, b_iota4], writes=[b_eq])
                    em.op("dve", lambda e: e.tensor_tensor(out=eq4, in0=eq4, in1=i16v[:, :, half, :].unsqueeze(2).to_broadcast([128, 8, 16, 16]),
                                                           op=ALU.mult), reads=[b_eq, b_i16f], writes=[b_eq])
                    em.op("dve", lambda e: e.tensor_reduce(out=isel[:, half, :], in_=eq[:].rearrange("p (r a) -> p r a", a=16), axis=AX.X,
                                                           op=ALU.add), reads=[b_eq], writes=[b_isel], acc=(half > 0))
                em.op("dve", lambda e: e.scalar_tensor_tensor(out=idxf[:], in0=isel[:, 0, :], scalar=128.0, in1=isel[:, 1, :], op0=ALU.mult,
                                                               op1=ALU.add), reads=[b_isel], writes=[b_idxf])
                yield
                k = next_pp()
                em.op("pe", lambda e: e.transpose(out=pP[k][:, 0:128], in_=idxf[:], identity=identf[:]), reads=[b_idxf, b_identf],
                      writes=[b_pP[k]])
                em.op("pe", lambda e: e.transpose(out=pP[k][:, 128:256], in_=gate[:], identity=identf[:]), reads=[b_gate, b_identf],
                      writes=[b_pP[k]], acc=True)
                em.op("dve", lambda e: e.tensor_copy(out=idxT[m % 3][:], in_=pP[k][:, 0:128]), reads=[b_pP[k]], writes=[b_idxT[m % 3]])
                em.op("dve", lambda e: e.tensor_copy(out=gateT[m % 3][:], in_=pP[k][:, 128:256]), reads=[b_pP[k]], writes=[b_gateT[m % 3]])

            def stage_P(m):
                for _ in stage_P_gen(m):
                    pass

            def U_gather(m, t):
                k = cnt["gu"] % NGU
                cnt["gu"] += 1
                em.dma("pool", lambda e: e.indirect_dma_start(out=ug[k][:], out_offset=None, in_=ub[:, :],
                                                              in_offset=bass.IndirectOffsetOnAxis(ap=idxT[m % 3][:, t:t + 1], axis=0)),
                       reads=[b_idxT[m % 3], b_ub], writes=[b_ug[k]])
                return k

            def U_part(m, t, k, half):
                kb = cnt["bq"] % 2
                cnt["bq"] += 1
                for c2 in range(2):
                    col = half * 1024 + c2 * 512
                    em.op("pe", lambda e: e.matmul(bq[kb][:, c2 * 512:(c2 + 1) * 512], lhsT=identb[:, t:t + 1].to_broadcast([128, 128]),
                                                   rhs=h2[m % 2][:, col:col + 512], start=True, stop=True),
                          reads=[b_identb, b_h2[m % 2]], writes=[b_bq[kb]], acc=(c2 > 0))
                em.op("dve", lambda e: e.scalar_tensor_tensor(out=scrE[:], in0=ug[k][:, half * 1024:(half + 1) * 1024], scalar=1.0,
                                                               in1=bq[kb][:], op0=ALU.mult, op1=ALU.mult, accum_out=hu4[:, half, t:t + 1]),
                      reads=[b_ug[k], b_bq[kb]], writes=[b_scrE, b_hu4], acc=True)

            def U_tok(m, t):
                k = U_gather(m, t)
                for c in range(2):
                    U_part(m, t, k, c)

            def U_start(m):
                em.op("dve", lambda e: e.memset(hu4[:], 0.0), writes=[b_hu4])

            def U_finish(m):
                em.op("dve", lambda e: e.tensor_tensor(out=hu[:], in0=hu4[:, 0, :], in1=hu4[:, 1, :], op=ALU.add), reads=[b_hu4], writes=[b_hu])
                em.op("act", lambda e: e.activation(out=hu[:], in_=hu[:], func=AF.Gelu), reads=[b_hu], writes=[b_hu])
                em.op("dve", lambda e: e.tensor_tensor(out=Aa[m % 2][:], in0=hu[:], in1=gateT[m % 3][:], op=ALU.mult),
                      reads=[b_hu, b_gateT[m % 3]], writes=[b_Aa[m % 2]])

            def V_gather(m, t):
                sb = t // 32
                n = 4 * m + sb
                ad = AD[n % 2]
                b_ad = b_AD[n % 2]
                if t % 32 == 0:
                    if n >= 2:
                        sbo = (n - 2) % 4
                        em.op("dve", lambda e: e.memset(ad[:, 32 * sbo:32 * sbo + 129 * 31 + 1:129], 0.0), writes=[b_ad])
                    em.op("dve", lambda e: e.tensor_copy(out=ad[:, 32 * sb:32 * sb + 129 * 31 + 1:129], in_=Aa[m % 2][:, sb * 32:(sb + 1) * 32]),
                          reads=[b_Aa[m % 2]], writes=[b_ad])
                k = cnt["gv"] % NGV
                cnt["gv"] += 1
                em.dma("pool", lambda e: e.indirect_dma_start(out=vg[k][:], out_offset=None, in_=vb[:, :],
                                                              in_offset=bass.IndirectOffsetOnAxis(ap=idxT[m % 3][:, t:t + 1], axis=0)),
                       reads=[b_idxT[m % 3], b_vb], writes=[b_vg[k]])
                return k

            def V_part(m, t, k, c):
                n = 4 * m + t // 32
                ad = AD[n % 2]
                b_ad = b_AD[n % 2]
                tl = t % 32
                em.op("pe", lambda e: e.matmul(pA[:, c * 512:(c + 1) * 512], lhsT=ad[:, tl * 128:(tl + 1) * 128],
                                               rhs=vg[k][:, c * 512:(c + 1) * 512], start=(t == 0), stop=(t == 127)),
                      reads=[b_ad, b_vg[k]], writes=[b_pA], acc=not (t == 0 and c == 0))

            def V_prefetch(m):
                em.dma("sp", lambda e: e.dma_start(out=xE[:], in_=x1s[m * 128:(m + 1) * 128, :]), writes=[b_xE])

            def V_epilogue(m):
                xx = xE
                b_xx = b_xE
                em.op("dve", lambda e: e.tensor_tensor(out=xx[:], in0=pA[:], in1=xx[:], op=ALU.add), reads=[b_pA, b_xx], writes=[b_xx])
                em.op("dve", lambda e: e.memset(stE[:], 0.0), writes=[b_stE])
                em.op("dve", lambda e: e.scalar_tensor_tensor(out=qb[:], in0=xx[:], scalar=1.0, in1=xx[:], op0=ALU.mult, op1=ALU.mult,
                                                               accum_out=stE[:, 8:9]), reads=[b_xx], writes=[b_qb, b_stE])
                cr = rstd_chain(stE, b_stE, 8, 9, 2048)
                em.op("dve", lambda e: e.scalar_tensor_tensor(out=xx[:], in0=xx[:], scalar=stE[:, cr:cr + 1], in1=gfb[:], op0=ALU.mult,
                                                               op1=ALU.mult), reads=[b_xx, b_stE, b_gfb], writes=[b_xx])
                em.dma("sp", lambda e: e.dma_start(out=y[m * 128:(m + 1) * 128, :], in_=xx[:]), reads=[b_xx])

            stage_P(0)
            stage_P(1)
            U_start(0)
            for t in range(128):
                U_tok(0, t)
            U_finish(0)
            for m in range(NOWN):
                pgen = stage_P_gen(m + 2) if m + 2 < NOWN else None
                if m + 1 < NOWN:
                    U_start(m + 1)
                V_prefetch(m)
                for t in range(128):
                    if pgen is not None and t >= 4 and t % 2 == 0:
                        if next(pgen, "done") == "done":
                            pgen = None
                    ku = U_gather(m + 1, t) if m + 1 < NOWN else None
                    kv = V_gather(m, t)
                    for c in range(4):
                        if ku is not None and c % 2 == 0:
                            U_part(m + 1, t, ku, c // 2)
                        V_part(m, t, kv, c)
                if pgen is not None:
                    for _ in pgen:
                        pass
                if m + 1 < NOWN:
                    U_finish(m + 1)
                V_epilogue(m)
            em.barrier()
        print("instructions:", em.ninst, "semaphores:", em.nsem)
    return nc


def _prep_inputs(x, positions, g_norm1, w_in, g_q_a, w_uq, g_kv_a, w_ukv, sgu_ln_g, sgu_ln_b, w_spatial, b_spatial,
                 b_gate, w_out, g_norm2, w_peer_q, peer_keys, peer_u, peer_v, g_final):
    f = lambda a: np.ascontiguousarray(np.asarray(a, dtype=np.float32))
    x = f(x)
    positions = np.asarray(positions).astype(np.int32)
    rep = lambda v: np.ascontiguousarray(np.broadcast_to(f(v).reshape(1, -1), (128, f(v).size)))
    pk = lambda v, n: np.ascontiguousarray(f(v).reshape(n, 128).T)
    freqs = (10000.0 ** (-np.arange(0, 64, 2, dtype=np.float32) / np.float32(64))).astype(np.float32)
    iota4 = np.ascontiguousarray(np.broadcast_to(np.tile(np.arange(16, dtype=np.float32), 128).reshape(1, 2048), (128, 2048)))
    kk = np.arange(128)
    tri = (kk[:, None] <= kk[None, :]).astype(np.float32)
    shared = {
        "freqs": rep(freqs),
        "ident": np.eye(128, dtype=np.float32),
        "iota4": iota4,
        "g1": pk(g_norm1[0], 16),
        "w_in": f(w_in[0]),
        "gq": pk(g_q_a[0], 4),
        "gkv": pk(g_kv_a[0], 4),
        "w_uq": f(w_uq[0]).reshape(512, 3072),
        "w_ukv": f(w_ukv[0]).reshape(512, 4096),
        "lng": rep(sgu_ln_g[0]),
        "lnb": rep(sgu_ln_b[0]),
        "wspT": np.ascontiguousarray(f(w_spatial[0]).transpose(2, 0, 1)),
        "triT": tri,
        "bsp": np.ascontiguousarray(f(b_spatial[0]).T),
        "bgate": rep(b_gate[0]),
        "w_out": f(w_out[0]),
        "g2b": rep(g_norm2[0]),
        "w_pq": f(w_peer_q[0]).reshape(2048, 2048),
        "keysT": np.ascontiguousarray(f(peer_keys[0]).reshape(16, 128, 128).transpose(2, 0, 1)),
        "peer_u": f(peer_u[0]),
        "peer_v": f(peer_v[0]),
        "gfb": rep(g_final),
    }
    in_maps = []
    for c in range(8):
        b, j = c // 4, c % 4
        blocks = [4 * m + j for m in range(NOWN)]
        xbat = x[b]
        xo = np.ascontiguousarray(xbat.reshape(NBA, 128, 2048)[blocks].reshape(NOWN * 128, 2048))
        posb = np.ascontiguousarray(positions[b].reshape(NBA, 128).T)
        poso = np.ascontiguousarray(positions[b].reshape(NBA, 128)[blocks].T)
        cm = np.zeros((128, 4, 128), np.float32)
        for i in range(4):
            if i < j:
                cm[:, i, :] = 1.0
            elif i == j:
                cm[:, i, :] = tri
        d = dict(shared)
        d.update({"xb": xbat, "xo": xo, "posb": posb, "poso": poso, "cmask": cm.reshape(128, 512)})
        in_maps.append(d)
    return in_maps


def kernel(**inputs):
    in_maps = _prep_inputs(**inputs)
    nc = build_nc()
    res = run_bass_kernel_spmd(nc, in_maps, core_ids=list(range(8)))
    out = np.empty((2, NBA * 128, 2048), np.float32)
    for c in range(8):
        b, j = c // 4, c % 4
        yc = np.asarray(res.results[c]["y"]).reshape(NOWN, 128, 2048)
        for m in range(NOWN):
            blk = 4 * m + j
            out[b, blk * 128:(blk + 1) * 128] = yc[m]
    return out
```

```python
import math
from contextlib import ExitStack

import numpy as np
import concourse.bass as bass
import concourse.mybir as mybir
from concourse.bass_utils import run_bass_kernel_spmd

F32 = mybir.dt.float32
BF16 = mybir.dt.bfloat16
U32 = mybir.dt.uint32
I32 = mybir.dt.int32
AF = mybir.ActivationFunctionType
ALU = mybir.AluOpType
AX = mybir.AxisListType

EPS = 1e-6
SEM_LIMIT = 24000
NBA = 64
NOWN = 16
NH = 16
QSCALE = 192.0 ** -0.5
TWO_PI = 2.0 * math.pi
C1 = 6.28125
C2 = TWO_PI - C1
PI_SAFE = 3.1415920


class Buf:
    __slots__ = ("name", "lw", "rd", "dsem", "dcnt")

    def __init__(self, name):
        self.name = name
        self.lw = []
        self.rd = []
        self.dsem = None
        self.dcnt = 0


def _compact(evs):
    best = {}
    for s, v in evs:
        k = id(s)
        if k not in best or best[k][1] < v:
            best[k] = (s, v)
    return list(best.values())


class Em:
    def __init__(self, nc, stack):
        self.nc = nc
        self.stack = stack
        self.engs = {"pe": nc.tensor, "act": nc.scalar, "dve": nc.vector, "pool": nc.gpsimd, "sp": nc.sync}
        self.sem = {}
        self.cnt = {}
        self.nsem = 0
        for k in self.engs:
            self.sem[k] = self._newsem("s_" + k)
            self.cnt[k] = 0
        self.waited = {k: {} for k in self.engs}
        self.dbufs = []
        self.old = []
        self.ninst = 0

    def _newsem(self, name):
        self.nsem += 1
        return self.stack.enter_context(self.nc.semaphore(f"{name}_{self.nsem}"))

    def buf(self, name):
        return Buf(name)

    def bufs(self, name, n):
        return [Buf(f"{name}{i}") for i in range(n)]

    def _wait(self, eng, events):
        w = self.waited[eng]
        best = {}
        for (s, v) in events:
            k = id(s)
            if w.get(k, 0) < v and (k not in best or best[k][1] < v):
                best[k] = (s, v)
        for k, (s, v) in best.items():
            self.engs[eng].wait_ge(s, v)
            w[k] = v

    def op(self, eng, fn, reads=(), writes=(), acc=False):
        ev = []
        for b in reads:
            ev.extend(b.lw)
        for b in writes:
            ev.extend(b.rd)
            ev.extend(b.lw)
        if eng == "pe":
            mine = id(self.sem["pe"])
            ev = [e for e in ev if id(e[0]) != mine]
        self._wait(eng, ev)
        if self.cnt[eng] >= SEM_LIMIT:
            self.old.append((self.sem[eng], self.cnt[eng]))
            self.sem[eng] = self._newsem("s_" + eng)
            self.cnt[eng] = 0
        ins = fn(self.engs[eng])
        self.cnt[eng] += 1
        ins.then_inc(self.sem[eng], 1)
        e = (self.sem[eng], self.cnt[eng])
        for b in reads:
            b.rd.append(e)
            if len(b.rd) > 16:
                b.rd = _compact(b.rd)
        for b in writes:
            if acc:
                b.lw.append(e)
                if len(b.lw) > 16:
                    b.lw = _compact(b.lw)
            else:
                b.lw = [e]
                b.rd = []
        self.ninst += 1
        return ins

    def dma(self, q, fn, reads=(), writes=(), sembuf=None, group=False):
        sb = sembuf or (writes[0] if writes else reads[0])
        ev = []
        for b in reads:
            ev.extend(b.lw)
        for b in writes:
            ev.extend(b.rd)
            if not group:
                ev.extend(b.lw)
        self._wait(q, ev)
        if sb.dsem is None or sb.dcnt >= SEM_LIMIT:
            if sb.dsem is not None:
                self.old.append((sb.dsem, sb.dcnt))
            else:
                self.dbufs.append(sb)
            sb.dsem = self._newsem("d_" + sb.name)
            sb.dcnt = 0
        ins = fn(self.engs[q])
        sb.dcnt += 16
        ins.then_inc(sb.dsem, 16)
        e = (sb.dsem, sb.dcnt)
        for b in reads:
            b.rd.append(e)
            if len(b.rd) > 16:
                b.rd = _compact(b.rd)
        for b in writes:
            if group:
                b.lw.append(e)
                if len(b.lw) > 16:
                    b.lw = _compact(b.lw)
            else:
                b.lw = [e]
                b.rd = []
        self.ninst += 1
        return ins

    def all_events(self):
        ev = [(self.sem[k], self.cnt[k]) for k in self.engs if self.cnt[k] > 0]
        ev += [(b.dsem, b.dcnt) for b in self.dbufs if b.dcnt > 0]
        ev += self.old
        return ev

    def barrier(self, engines=("pe", "act", "dve", "pool", "sp")):
        ev = self.all_events()
        for e in engines:
            self._wait(e, ev)
        self.old = []


def build_nc(stop=None, dbg=False):
    nc = bass.Bass("TRN2", target_bir_lowering=False)

    def din(name, shape, dt=F32):
        return nc.dram_tensor(name, shape, dt, kind="ExternalInput").ap()

    xb = din("xb", [NBA * 128, 2048])
    xo = din("xo", [NOWN * 128, 2048])
    posb = din("posb", [128, NBA], I32)
    poso = din("poso", [128, NOWN], I32)
    cmask_d = din("cmask", [128, 512])
    freqs_d = din("freqs", [128, 32])
    ident_d = din("ident", [128, 128])
    iota4_d = din("iota4", [128, 2048])
    g1_d = din("g1", [128, 16])
    w_in = din("w_in", [2048, 9280])
    gq_d = din("gq", [128, 4])
    gkv_d = din("gkv", [128, 4])
    w_uq = din("w_uq", [512, 3072])
    w_ukv = din("w_ukv", [512, 4096])
    lng_d = din("lng", [128, 2048])
    lnb_d = din("lnb", [128, 2048])
    wspT_d = din("wspT", [128, 8, 128])
    triT_d = din("triT", [128, 128])
    bsp_d = din("bsp", [128, 8])
    bgate_d = din("bgate", [128, 4096])
    w_out = din("w_out", [2048, 2048])
    g2_d = din("g2b", [128, 2048])
    w_pq = din("w_pq", [2048, 2048])
    keysT_d = din("keysT", [128, 16, 128])
    peer_u = din("peer_u", [16384, 2048])
    peer_v = din("peer_v", [16384, 2048])
    gf_d = din("gfb", [128, 2048])
    y = nc.dram_tensor("y", [NOWN * 128, 2048], F32, kind="ExternalOutput").ap()

    skind = "ExternalOutput" if dbg else "Internal"
    zs = nc.dram_tensor("zs", [NOWN * 128, 4096], BF16, kind=skind).ap()
    gs = nc.dram_tensor("gs", [NOWN * 128, 4096], BF16, kind=skind).ap()
    qrs = nc.dram_tensor("qrs", [128, 8, NOWN * 128], BF16, kind=skind).ap()
    oas = nc.dram_tensor("oas", [NOWN * 128, 2048], BF16, kind=skind).ap()
    x1s = nc.dram_tensor("x1s", [NOWN * 128, 2048], F32, kind=skind).ap()
    ub = nc.dram_tensor("ub", [16384, 2048], BF16).ap()
    vb = nc.dram_tensor("vb", [16384, 2048], BF16).ap()
    if dbg:
        d_cqnT = nc.dram_tensor("d_cqnT", [128, 4, NOWN * 128], BF16, kind="ExternalOutput").ap()
        d_ckvnT = nc.dram_tensor("d_ckvnT", [128, 4, NBA * 128], BF16, kind="ExternalOutput").ap()
        d_kpeT = nc.dram_tensor("d_kpeT", [128, NBA * 128], BF16, kind="ExternalOutput").ap()

    with ExitStack() as gst:
        em = Em(nc, gst)
        b_ub = em.buf("ub")
        b_vb = em.buf("vb")
        tab_jobs = [(peer_u, ub, b_ub, i) for i in range(32)] + [(peer_v, vb, b_vb, i) for i in range(32)]

        def emit_tab_jobs(n):
            for _ in range(n):
                if not tab_jobs:
                    return
                src, dst, bb, i = tab_jobs.pop(0)
                em.dma("pool", lambda e: e.dma_start(out=dst[i * 512:(i + 1) * 512, :], in_=src[i * 512:(i + 1) * 512, :]), writes=[bb], group=True)

        def T(st, name, shape, dt):
            return st.enter_context(nc.sbuf_tensor("sb_" + name, shape, dt))

        def P(st, name, shape, dt=F32):
            return st.enter_context(nc.psum_tensor("ps_" + name, shape, dt))

        identf = T(gst, "identf", [128, 128], F32)
        identb = T(gst, "identb", [128, 128], BF16)
        b_identf = em.buf("identf")
        b_identb = em.buf("identb")
        em.dma("sp", lambda e: e.dma_start(out=identf[:], in_=ident_d[:, :]), writes=[b_identf])
        em.op("dve", lambda e: e.tensor_copy(out=identb[:], in_=identf[:]), reads=[b_identf], writes=[b_identb])

        def load_small(st, name, src, shape, dt=F32):
            t = T(st, name, shape, dt)
            b = em.buf(name)
            em.dma("sp", lambda e: e.dma_start(out=t[:], in_=src), writes=[b])
            return t, b


        def rstd_chain(st_t, b_st, c_ss, c0, n):
            em.op("dve", lambda e: e.tensor_scalar(out=st_t[:, c0:c0 + 1], in0=st_t[:, c_ss:c_ss + 1], scalar1=1.0 / n,
                                                   scalar2=EPS, op0=ALU.mult, op1=ALU.add), reads=[b_st], writes=[b_st])
            em.op("act", lambda e: e.activation(out=st_t[:, c0 + 1:c0 + 2], in_=st_t[:, c0:c0 + 1], func=AF.Sqrt),
                  reads=[b_st], writes=[b_st])
            em.op("dve", lambda e: e.reciprocal(out=st_t[:, c0 + 2:c0 + 3], in_=st_t[:, c0 + 1:c0 + 2]),
                  reads=[b_st], writes=[b_st])
            return c0 + 2

        def sincos_tables(st, name, pos_src, nblk, scale, want_b=True):
            n = nblk * 64
            csA = T(st, name + "_csA", [128, nblk, 64], F32)
            csB = T(st, name + "_csB", [128, nblk, 64], F32) if want_b else None
            b = em.buf(name + "_cs")
            with ExitStack() as tmp:
                posi, b_posi = load_small(tmp, name + "_pi", pos_src, [128, nblk], I32)
                frq, b_frq = load_small(tmp, name + "_fr", freqs_d[:, :], [128, 32])
                posf = T(tmp, name + "_pf", [128, nblk], F32)
                A2 = T(tmp, name + "_A2", [128, nblk, 64], F32)
                kf = T(tmp, name + "_kf", [128, n], F32)
                ki = T(tmp, name + "_ki", [128, n], I32)
                A2f = A2[:].rearrange("p b e -> p (b e)")
                csAf = csA[:].rearrange("p b e -> p (b e)")
                em.op("dve", lambda e: e.tensor_copy(out=posf[:], in_=posi[:]), reads=[b_posi], writes=[b])
                em.op("dve", lambda e: e.tensor_tensor(out=A2[:, :, 32:64], in0=posf[:].unsqueeze(2).to_broadcast([128, nblk, 32]),
                                                       in1=frq[:].unsqueeze(1).to_broadcast([128, nblk, 32]), op=ALU.mult),
                      reads=[b, b_frq], writes=[b])
                em.op("dve", lambda e: e.tensor_scalar(out=A2[:, :, 0:32], in0=A2[:, :, 32:64], scalar1=math.pi / 2, scalar2=None,
                                                       op0=ALU.add), reads=[b], writes=[b])
                em.op("dve", lambda e: e.tensor_scalar(out=kf[:], in0=A2f, scalar1=1.0 / TWO_PI, scalar2=None, op0=ALU.mult),
                      reads=[b], writes=[b])
                em.op("dve", lambda e: e.tensor_copy(out=ki[:], in_=kf[:]), reads=[b], writes=[b])
                em.op("dve", lambda e: e.tensor_copy(out=kf[:], in_=ki[:]), reads=[b], writes=[b])
                em.op("dve", lambda e: e.scalar_tensor_tensor(out=A2f, in0=kf[:], scalar=-C1, in1=A2f, op0=ALU.mult, op1=ALU.add),
                      reads=[b], writes=[b])
                em.op("dve", lambda e: e.scalar_tensor_tensor(out=A2f, in0=kf[:], scalar=-C2, in1=A2f, op0=ALU.mult, op1=ALU.add),
                      reads=[b], writes=[b])
                em.op("dve", lambda e: e.tensor_scalar(out=kf[:], in0=A2f, scalar1=math.pi, scalar2=None, op0=ALU.is_gt),
                      reads=[b], writes=[b])
                em.op("dve", lambda e: e.scalar_tensor_tensor(out=A2f, in0=kf[:], scalar=-TWO_PI, in1=A2f, op0=ALU.mult, op1=ALU.add),
                      reads=[b], writes=[b])
                em.op("dve", lambda e: e.tensor_scalar(out=kf[:], in0=A2f, scalar1=-math.pi, scalar2=None, op0=ALU.is_lt),
                      reads=[b], writes=[b])
                em.op("dve", lambda e: e.scalar_tensor_tensor(out=A2f, in0=kf[:], scalar=TWO_PI, in1=A2f, op0=ALU.mult, op1=ALU.add),
                      reads=[b], writes=[b])
                em.op("dve", lambda e: e.tensor_scalar(out=A2f, in0=A2f, scalar1=PI_SAFE, scalar2=-PI_SAFE, op0=ALU.min, op1=ALU.max),
                      reads=[b], writes=[b])
                em.op("act", lambda e: e.activation(out=csAf, in_=A2f, func=AF.Sin), reads=[b], writes=[b])
                if scale != 1.0:
                    em.op("dve", lambda e: e.tensor_scalar(out=csAf, in0=csAf, scalar1=scale, scalar2=None, op0=ALU.mult),
                          reads=[b], writes=[b])
                if want_b:
                    em.op("dve", lambda e: e.tensor_copy(out=csB[:, :, 0:32], in_=csA[:, :, 32:64]), reads=[b], writes=[b])
                    em.op("dve", lambda e: e.tensor_copy(out=csB[:, :, 32:64], in_=csA[:, :, 0:32]), reads=[b], writes=[b])
                em.barrier()
            return csA, csB, b

        with ExitStack() as phAB:
            cqnT = T(phAB, "cqnT", [128, 4, NOWN * 128], BF16)
            b_cqnT = em.bufs("cqnT", NOWN)
            with ExitStack() as ph:
                g1, b_g1 = load_small(ph, "g1", g1_d[:, :], [128, 16])
                gq, b_gq = load_small(ph, "gq", gq_d[:, :], [128, 4])
                csAo, csBo, b_cso = sincos_tables(ph, "cso", poso[:, :], NOWN, QSCALE)

                hT = T(ph, "hT_own", [128, 16, NOWN * 128], BF16)
                b_hT = em.bufs("hTo", NOWN)
                stt = [T(ph, f"Ast{i}", [128, 8], F32) for i in range(2)]
                b_stt = em.bufs("Ast", 2)
                junk = T(ph, "Ajunk", [128, 2048], BF16)
                b_junk = em.buf("Ajunk")
                tpp = P(ph, "Atp", [128, 16, 128], BF16)
                b_tpp = em.buf("Atp")
                pacc = [P(ph, f"Apacc{i}", [128, 512], F32) for i in range(2)]
                b_pacc = em.bufs("Apacc", 2)
                pqr = P(ph, "Apqr", [128, 1024], F32)
                b_pqr = em.buf("Apqr")

                with ExitStack() as a0:
                    xt = [T(a0, f"Ax{i}", [128, 2048], F32) for i in range(2)]
                    b_xt = em.bufs("Ax", 2)
                    xs = [T(a0, f"Axs{i}", [128, 2048], BF16) for i in range(2)]
                    b_xs = em.bufs("Axs", 2)
                    def A0_a(m):
                        i = m % 2
                        em.dma("sp", lambda e: e.dma_start(out=xt[i][:], in_=xo[m * 128:(m + 1) * 128, :]), writes=[b_xt[i]])
                        em.op("act", lambda e: e.activation(out=junk[:], in_=xt[i][:], func=AF.Square, accum_out=stt[i][:, 0:1]),
                              reads=[b_xt[i]], writes=[b_junk, b_stt[i]])
                        cr = rstd_chain(stt[i], b_stt[i], 0, 1, 2048)
                        em.op("dve", lambda e: e.tensor_scalar(out=xs[i][:], in0=xt[i][:], scalar1=stt[i][:, cr:cr + 1], scalar2=None, op0=ALU.mult),
                              reads=[b_xt[i], b_stt[i]], writes=[b_xs[i]])

                    def A0_b(m):
                        i = m % 2
                        for kc in range(16):
                            em.op("pe", lambda e: e.transpose(out=tpp[:, kc, :], in_=xs[i][:, kc * 128:(kc + 1) * 128], identity=identb[:]),
                                  reads=[b_xs[i], b_identb], writes=[b_tpp], acc=(kc > 0))
                        em.op("dve", lambda e: e.tensor_tensor(out=hT[:, :, m * 128:(m + 1) * 128], in0=tpp[:],
                                                               in1=g1[:].unsqueeze(2).to_broadcast([128, 16, 128]), op=ALU.mult),
                              reads=[b_tpp, b_g1], writes=[b_hT[m]])

                    A0_a(0)
                    for m in range(NOWN):
                        if m + 1 < NOWN:
                            A0_a(m + 1)
                        A0_b(m)
                    em.barrier()

                a1 = ph
                wst = T(a1, "Awst", [128, 16, 512], F32)
                b_wst = em.buf("Awst")
                wbf = [T(a1, f"Awbf{i}", [128, 16, 512], BF16) for i in range(2)]
                b_wbf = em.bufs("Awbf", 2)
                ost = [T(a1, f"Aost{i}", [128, 512], BF16) for i in range(3)]
                b_ost = em.bufs("Aost", 3)
                gtmp = [T(a1, f"Agt{i}", [128, 512], F32) for i in range(2)]
                b_gtmp = em.bufs("Agt", 2)
                bgt = [T(a1, f"Abg{i}", [128, 512], F32) for i in range(2)]
                b_bgt = em.bufs("Abg", 2)

                chunks = [("cq", 0)] + [("z", 1088 + 512 * i) for i in range(8)] + [("g", 5184 + 512 * i) for i in range(8)]
                cvt_engs = ["dve", "pool"]

                def load_chunk_dma(ci):
                    kind, c0 = chunks[ci]
                    em.dma("sp", lambda e: e.dma_start(out=wst[:], in_=w_in[:, c0:c0 + 512].rearrange("(kc p) n -> p kc n", p=128)),
                           writes=[b_wst])
                    if kind == "g":
                        gc = c0 - 5184
                        em.dma("sp", lambda e: e.dma_start(out=bgt[ci % 2][:], in_=bgate_d[:, gc:gc + 512]), writes=[b_bgt[ci % 2]])

                def load_chunk_cvt(ci):
                    wb = wbf[ci % 2]
                    for hh in range(2):
                        eng = cvt_engs[hh]
                        em.op(eng, lambda e: e.tensor_copy(out=wb[:, hh * 8:(hh + 1) * 8, :], in_=wst[:, hh * 8:(hh + 1) * 8, :]),
                              reads=[b_wst], writes=[b_wbf[ci % 2]], acc=(hh > 0))

                load_chunk_dma(0)
                load_chunk_cvt(0)
                oi = 0
                gi = 0
                pi_ = 0
                for ci, (kind, c0) in enumerate(chunks):
                    if ci + 1 < len(chunks):
                        load_chunk_dma(ci + 1)
                    wb = wbf[ci % 2]
                    b_wb = b_wbf[ci % 2]
                    for m in range(NOWN):
                        if m == 8 and ci + 1 < len(chunks):
                            load_chunk_cvt(ci + 1)
                        pa = pacc[pi_ % 2]
                        b_pa = b_pacc[pi_ % 2]
                        pi_ += 1
                        for kc in range(16):
                            em.op("pe", lambda e: e.matmul(pa[:], lhsT=hT[:, kc, m * 128:(m + 1) * 128], rhs=wb[:, kc, :],
                                                           start=(kc == 0), stop=(kc == 15)),
                                  reads=[b_hT[m], b_wb], writes=[b_pa], acc=(kc > 0))
                        if kind == "cq":
                            i = m % 2
                            em.op("act", lambda e: e.activation(out=junk[:, 0:512], in_=pa[:], func=AF.Square, accum_out=stt[i][:, 4:5]),
                                  reads=[b_pa], writes=[b_junk, b_stt[i]])
                            cr = rstd_chain(stt[i], b_stt[i], 4, 5, 512)
                            o = ost[oi % 3]
                            b_o = b_ost[oi % 3]
                            oi += 1
                            em.op("dve", lambda e: e.tensor_scalar(out=o[:], in0=pa[:], scalar1=stt[i][:, cr:cr + 1], scalar2=None, op0=ALU.mult),
                                  reads=[b_pa, b_stt[i]], writes=[b_o])
                            for rc in range(4):
                                em.op("pe", lambda e: e.transpose(out=tpp[:, rc, :], in_=o[:, rc * 128:(rc + 1) * 128], identity=identb[:]),
                                      reads=[b_o, b_identb], writes=[b_tpp], acc=(rc > 0))
                            em.op("dve", lambda e: e.tensor_tensor(out=cqnT[:, :, m * 128:(m + 1) * 128], in0=tpp[:, 0:4, :],
                                                                   in1=gq[:].unsqueeze(2).to_broadcast([128, 4, 128]), op=ALU.mult),
                                  reads=[b_tpp, b_gq], writes=[b_cqnT[m]])
                        elif kind == "z":
                            o = ost[oi % 3]
                            b_o = b_ost[oi % 3]
                            oi += 1
                            em.op("act", lambda e: e.activation(out=o[:], in_=pa[:], func=AF.Gelu), reads=[b_pa], writes=[b_o])
                            zc = c0 - 1088
                            em.dma("pool", lambda e: e.dma_start(out=zs[m * 128:(m + 1) * 128, zc:zc + 512], in_=o[:]), reads=[b_o])
                        else:
                            gc = c0 - 5184
                            gt = gtmp[gi % 2]
                            b_gt = b_gtmp[gi % 2]
                            gi += 1
                            em.op("dve", lambda e: e.tensor_tensor(out=gt[:], in0=pa[:], in1=bgt[ci % 2][:], op=ALU.add),
                                  reads=[b_pa, b_bgt[ci % 2]], writes=[b_gt])
                            o = ost[oi % 3]
                            b_o = b_ost[oi % 3]
                            oi += 1
                            em.op("act", lambda e: e.activation(out=o[:], in_=gt[:], func=AF.Sigmoid), reads=[b_gt], writes=[b_o])
                            em.dma("pool", lambda e: e.dma_start(out=gs[m * 128:(m + 1) * 128, gc:gc + 512], in_=o[:]), reads=[b_o])

                wqr_st = wst[:, 0:8, :].rearrange("p r n -> p (r n)").rearrange("p (r h e) -> p r h e", r=4, h=16)
                wqr = wbf[0][:, 0:8, :].rearrange("p r n -> p (r n)").rearrange("p (r h e) -> p r h e", r=4, h=16)
                w_uq_v = w_uq.rearrange("(rc p) (h e) -> p rc h e", p=128, e=192)
                for rc in range(4):
                    em.dma("sp", lambda e: e.dma_start(out=wqr_st[:, rc, :, :], in_=w_uq_v[:, rc, :, 128:192]), writes=[b_wst],
                           group=(rc > 0))
                em.op("dve", lambda e: e.tensor_copy(out=wqr, in_=wqr_st), reads=[b_wst], writes=[b_wbf[0]])
                wqr2 = wbf[0][:, 0:8, :].rearrange("p r n -> p (r n)").rearrange("p (r n) -> p r n", r=4)
                ra = T(ph, "Ara", [128, 16, 64], F32)
                rb = T(ph, "Arb", [128, 16, 64], F32)
                b_rab = em.buf("Arab")
                qr = [T(ph, f"Aqr{i}", [128, 16, 64], BF16) for i in range(2)]
                b_qr = em.bufs("Aqr", 2)
                qrT = [T(ph, f"AqrT{i}", [128, 8, 128], BF16) for i in range(2)]
                b_qrT = em.bufs("AqrT", 2)
                pqr3 = pqr[:].rearrange("p (h e) -> p h e", e=64)
                for m in range(NOWN):
                    i = m % 2
                    for half in range(2):
                        for rc in range(4):
                            em.op("pe", lambda e: e.matmul(pqr[:, half * 512:(half + 1) * 512], lhsT=cqnT[:, rc, m * 128:(m + 1) * 128],
                                                           rhs=wqr2[:, rc, half * 512:(half + 1) * 512], start=(rc == 0), stop=(rc == 3)),
                                  reads=[b_cqnT[m], b_wbf[0]], writes=[b_pqr], acc=not (half == 0 and rc == 0))
                    cA = csAo[:, m, :].unsqueeze(1).to_broadcast([128, 16, 64])
                    cB = csBo[:, m, :].unsqueeze(1).to_broadcast([128, 16, 64])
                    em.op("dve", lambda e: e.tensor_tensor(out=ra[:], in0=pqr3, in1=cA, op=ALU.mult), reads=[b_pqr, b_cso], writes=[b_rab])
                    em.op("dve", lambda e: e.tensor_tensor(out=rb[:], in0=pqr3, in1=cB, op=ALU.mult), reads=[b_pqr, b_cso], writes=[b_rab],
                          acc=True)
                    em.op("dve", lambda e: e.tensor_tensor(out=qr[i][:, :, 0:32], in0=ra[:, :, 0:32], in1=ra[:, :, 32:64], op=ALU.subtract),
                          reads=[b_rab], writes=[b_qr[i]])
                    em.op("dve", lambda e: e.tensor_tensor(out=qr[i][:, :, 32:64], in0=rb[:, :, 0:32], in1=rb[:, :, 32:64], op=ALU.add),
                          reads=[b_rab], writes=[b_qr[i]], acc=True)
                    qrf = qr[i][:].rearrange("p h e -> p (h e)")
                    for pr in range(8):
                        em.op("pe", lambda e: e.transpose(out=tpp[:, pr, :], in_=qrf[:, pr * 128:(pr + 1) * 128], identity=identb[:]),
                              reads=[b_qr[i], b_identb], writes=[b_tpp], acc=(pr > 0))
                    em.op("act", lambda e: e.activation(out=qrT[i][:], in_=tpp[:, 0:8, :], func=AF.Copy), reads=[b_tpp], writes=[b_qrT[i]])
                    em.dma("pool", lambda e: e.dma_start(out=qrs[:, :, m * 128:(m + 1) * 128], in_=qrT[i][:]), reads=[b_qrT[i]])
                em.barrier()
            if dbg:
                em.dma("sp", lambda e: e.dma_start(out=d_cqnT[:, :, :], in_=cqnT[:]), reads=list(b_cqnT))
            if stop == "A":
                em.barrier()
                return nc

            with ExitStack() as phLB:
                ckvnT = T(phLB, "ckvnT", [128, NBA, 4, 128], BF16)
                b_ckvnT = em.bufs("ckvnT", NBA)
                kpeT = T(phLB, "kpeT", [128, NBA * 128], BF16)
                b_kpeT = em.bufs("kpeT", NBA)

                with ExitStack() as ph:
                    g1, b_g1 = load_small(ph, "Lg1", g1_d[:, :], [128, 16])
                    gkv, b_gkv = load_small(ph, "Lgkv", gkv_d[:, :], [128, 4])
                    csA, _, b_cs = sincos_tables(ph, "csb", posb[:, :], NBA, 1.0, want_b=False)
                    if stop == "L0":
                        em.barrier()
                        return nc
                    wlat = T(ph, "wlat", [128, 16, 576], BF16)
                    b_wlat = em.buf("wlat")
                    with ExitStack() as tmp:
                        wst = T(tmp, "Lwst", [128, 16, 288], F32)
                        b_wst = em.buf("Lwst")
                        for hh in range(2):
                            c0 = 512 + hh * 288
                            em.dma("sp", lambda e: e.dma_start(out=wst[:], in_=w_in[:, c0:c0 + 288].rearrange("(kc p) n -> p kc n", p=128)),
                                   writes=[b_wst])
                            em.op("dve", lambda e: e.tensor_tensor(out=wlat[:, :, hh * 288:(hh + 1) * 288], in0=wst[:],
                                                                   in1=g1[:].unsqueeze(2).to_broadcast([128, 16, 288]), op=ALU.mult), reads=[b_wst, b_g1],
                                  writes=[b_wlat], acc=(hh > 0))
                        em.barrier()
                    if stop == "L1":
                        em.barrier()
                        return nc
                    xt = [T(ph, f"Lx{i}", [128, 2048], F32) for i in range(3)]
                    b_xt = em.bufs("Lx", 3)
                    xs = [T(ph, f"Lxs{i}", [128, 2048], BF16) for i in range(3)]
                    b_xs = em.bufs("Lxs", 3)
                    hTb = [T(ph, f"LhT{i}", [128, 16, 128], BF16) for i in range(2)]
                    b_hTb = em.bufs("LhT", 2)
                    junk = T(ph, "Ljunk", [128, 2048], BF16)
                    b_junk = em.buf("Ljunk")
                    junk2 = T(ph, "Ljunk2", [128, 512], BF16)
                    b_junk2 = em.buf("Ljunk2")
                    stt = [T(ph, f"Lst{i}", [128, 8], F32) for i in range(5)]
                    b_stt = em.bufs("Lst", 5)
                    st2 = [T(ph, f"Lsu{i}", [128, 8], F32) for i in range(2)]
                    b_st2 = em.bufs("Lsu", 2)
                    ckvn = [T(ph, f"Lckvn{i}", [128, 512], BF16) for i in range(2)]
                    b_ckvn = em.bufs("Lckvn", 2)
                    ra = T(ph, "Lra", [128, 64], F32)
                    rb = T(ph, "Lrb", [128, 64], F32)
                    b_rab = em.buf("Lrab")
                    kr = [T(ph, f"Lkr{i}", [128, 128], BF16) for i in range(2)]
                    b_kr = em.bufs("Lkr", 2)
                    tpp = P(ph, "Ltp", [128, 16, 128], BF16)
                    b_tpp = em.buf("Ltp")
                    tp2s = [P(ph, f"Ltp2_{i}", [128, 8, 128], BF16) for i in range(2)]
                    b_tp2s = em.bufs("Ltp2_", 2)
                    plat = [P(ph, f"Lplat{i}", [128, 512], F32) for i in range(2)]
                    b_plat = em.bufs("Lplat", 2)
                    plat2 = [P(ph, f"Lplatb{i}", [128, 512], F32)[:, 0:64] for i in range(2)]
                    b_plat2 = em.bufs("Lplatb", 2)
                    NBL = NBA

                    def L_S1(blk):
                        j3 = blk % 3
                        j5 = blk % 5
                        em.dma("sp", lambda e: e.dma_start(out=xt[j3][:], in_=xb[blk * 128:(blk + 1) * 128, :]), writes=[b_xt[j3]])
                        em.op("act", lambda e: e.activation(out=junk[:], in_=xt[j3][:], func=AF.Square, accum_out=stt[j5][:, 0:1]),
                              reads=[b_xt[j3]], writes=[b_junk, b_stt[j5]])
                        em.op("act", lambda e: e.activation(out=xs[j3][:], in_=xt[j3][:], func=AF.Copy), reads=[b_xt[j3]], writes=[b_xs[j3]])
                        rstd_chain(stt[j5], b_stt[j5], 0, 1, 2048)

                    def L_S2(blk):
                        j3 = blk % 3
                        i = blk % 2
                        for kc in range(16):
                            em.op("pe", lambda e: e.transpose(out=tpp[:, kc, :], in_=xs[j3][:, kc * 128:(kc + 1) * 128], identity=identb[:]),
                                  reads=[b_xs[j3], b_identb], writes=[b_tpp], acc=(kc > 0))
                        em.op("act", lambda e: e.activation(out=hTb[i][:], in_=tpp[:], func=AF.Copy), reads=[b_tpp], writes=[b_hTb[i]])

                    def L_S3(blk):
                        i = blk % 2
                        for kc in range(16):
                            em.op("pe", lambda e: e.matmul(plat[i][:], lhsT=hTb[i][:, kc, :], rhs=wlat[:, kc, 0:512], start=(kc == 0),
                                                           stop=(kc == 15)), reads=[b_hTb[i], b_wlat], writes=[b_plat[i]], acc=(kc > 0))
                        for kc in range(16):
                            em.op("pe", lambda e: e.matmul(plat2[i], lhsT=hTb[i][:, kc, :], rhs=wlat[:, kc, 512:576], start=(kc == 0),
                                                           stop=(kc == 15)), reads=[b_hTb[i], b_wlat], writes=[b_plat2[i]], acc=(kc > 0))
                        j5 = blk % 5
                        em.op("act", lambda e: e.activation(out=junk2[:], in_=plat[i][:], func=AF.Square, accum_out=st2[i][:, 4:5]),
                              reads=[b_plat[i]], writes=[b_junk2, b_st2[i]])
                        em.op("dve", lambda e: e.tensor_scalar(out=st2[i][:, 3:4], in0=st2[i][:, 4:5], scalar1=1.0 / 512, scalar2=None, op0=ALU.mult),
                              reads=[b_st2[i]], writes=[b_st2[i]])
                        em.op("dve", lambda e: e.scalar_tensor_tensor(out=st2[i][:, 5:6], in0=stt[j5][:, 1:2], scalar=EPS, in1=st2[i][:, 3:4],
                                                                       op0=ALU.mult, op1=ALU.add), reads=[b_stt[j5], b_st2[i]], writes=[b_st2[i]])
                        em.op("act", lambda e: e.activation(out=st2[i][:, 6:7], in_=st2[i][:, 5:6], func=AF.Sqrt), reads=[b_st2[i]], writes=[b_st2[i]])
                        em.op("dve", lambda e: e.reciprocal(out=st2[i][:, 7:8], in_=st2[i][:, 6:7]), reads=[b_st2[i]], writes=[b_st2[i]])

                    def L_S3b(blk):
                        i = blk % 2
                        cr2 = 7
                        tp2 = tp2s[i]
                        b_tp2 = b_tp2s[i]
                        em.op("dve", lambda e: e.tensor_scalar(out=ckvn[i][:], in0=plat[i][:], scalar1=st2[i][:, cr2:cr2 + 1], scalar2=None, op0=ALU.mult),
                              reads=[b_plat[i], b_st2[i]], writes=[b_ckvn[i]])
                        for rc in range(4):
                            em.op("pe", lambda e: e.transpose(out=tp2[:, rc, :], in_=ckvn[i][:, rc * 128:(rc + 1) * 128], identity=identb[:]),
                                  reads=[b_ckvn[i], b_identb], writes=[b_tp2], acc=(rc > 0))
                        j5 = blk % 5
                        rr = stt[j5][:, 3:4]
                        em.op("dve", lambda e: e.scalar_tensor_tensor(out=ra[:], in0=plat2[i], scalar=rr, in1=csA[:, blk, :], op0=ALU.mult, op1=ALU.mult),
                              reads=[b_plat2[i], b_cs, b_stt[j5]], writes=[b_rab])
                        em.op("dve", lambda e: e.scalar_tensor_tensor(out=rb[:, 0:32], in0=plat2[i][:, 0:32], scalar=rr, in1=csA[:, blk, 32:64],
                                                                       op0=ALU.mult, op1=ALU.mult), reads=[b_plat2[i], b_cs, b_stt[j5]], writes=[b_rab], acc=True)
                        em.op("dve", lambda e: e.scalar_tensor_tensor(out=rb[:, 32:64], in0=plat2[i][:, 32:64], scalar=rr, in1=csA[:, blk, 0:32],
                                                                       op0=ALU.mult, op1=ALU.mult), reads=[b_plat2[i], b_cs, b_stt[j5]], writes=[b_rab], acc=True)
                        em.op("dve", lambda e: e.tensor_tensor(out=kr[i][:, 0:32], in0=ra[:, 0:32], in1=ra[:, 32:64], op=ALU.subtract),
                              reads=[b_rab], writes=[b_kr[i]])
                        em.op("dve", lambda e: e.tensor_tensor(out=kr[i][:, 32:64], in0=rb[:, 0:32], in1=rb[:, 32:64], op=ALU.add),
                              reads=[b_rab], writes=[b_kr[i]], acc=True)
                        em.op("dve", lambda e: e.tensor_copy(out=kr[i][:, 64:128], in_=kr[i][:, 0:64]), reads=[b_kr[i]], writes=[b_kr[i]],
                              acc=True)
                        em.op("pe", lambda e: e.transpose(out=tp2[:, 4, :], in_=kr[i][:], identity=identb[:]),
                              reads=[b_kr[i], b_identb], writes=[b_tp2], acc=True)

                    def L_S4(blk):
                        tp2 = tp2s[blk % 2]
                        b_tp2 = b_tp2s[blk % 2]
                        em.op("dve", lambda e: e.tensor_copy(out=ckvnT[:, blk, :, :], in_=tp2[:, 0:4, :]),
                              reads=[b_tp2], writes=[b_ckvnT[blk]])
                        em.op("dve", lambda e: e.tensor_copy(out=kpeT[:, blk * 128:(blk + 1) * 128], in_=tp2[:, 4, :]),
                              reads=[b_tp2], writes=[b_kpeT[blk]])

                    for k in range(NBL + 4):
                        if k < NBL:
                            L_S1(k)
                        if 0 <= k - 1 < NBL:
                            L_S2(k - 1)
                        if 0 <= k - 2 < NBL:
                            L_S3(k - 2)
                        if 0 <= k - 3 < NBL:
                            L_S3b(k - 3)
                        if 0 <= k - 4 < NBL:
                            L_S4(k - 4)
                    em.barrier()
                if stop == "L6":
                    em.barrier()
                    return nc
                if dbg:
                    for rc in range(4):
                        for hh in range(4):
                            em.dma("sp", lambda e: e.dma_start(out=d_ckvnT[:, rc, hh * 2048:(hh + 1) * 2048].rearrange("p (b t) -> p b t", t=128), in_=ckvnT[:, hh * 16:(hh + 1) * 16, rc, :]),
                                   reads=list(b_ckvnT))
                    em.dma("sp", lambda e: e.dma_start(out=d_kpeT[:, :], in_=kpeT[:]), reads=list(b_kpeT))
                if stop == "L":
                    em.barrier()
                    return nc

                with ExitStack() as ph:
                    gkvB, b_gkvB = load_small(ph, "Bgkv", gkv_d[:, :], [128, 4])
                    cmf, b_cmf = load_small(ph, "cmf", cmask_d[:, :], [128, 512])
                    cm = T(ph, "cm", [128, 512], BF16)
                    b_cm = em.buf("cm")
                    em.op("dve", lambda e: e.tensor_copy(out=cm[:], in_=cmf[:]), reads=[b_cmf], writes=[b_cm])
                    wq_st = [T(ph, f"Bwqst{i}", [128, 4, 128], F32) for i in range(2)]
                    wkv_st = [T(ph, f"Bwkvst{i}", [128, 4, 256], F32) for i in range(2)]
                    b_wst = em.bufs("Bwst", 2)
                    wq_bf = [T(ph, f"Bwq{i}", [128, 4, 128], BF16) for i in range(2)]
                    wkv_bf = [T(ph, f"Bwkv{i}", [128, 4, 256], BF16) for i in range(2)]
                    b_wbf = em.bufs("Bwbf", 2)
                    KnT = T(ph, "KnT", [128, NBA * 128], BF16)
                    b_KnT = em.bufs("KnT", 16)
                    Vh = T(ph, "Vh", [128, NBA, 132], BF16)
                    b_Vh = em.bufs("Vh", 16)
                    b_Vones = em.buf("Vones")
                    qnT = T(ph, "qnT", [128, NOWN * 128], BF16)
                    b_qnT = em.bufs("qnT", 4)
                    qrTh = [T(ph, f"qrTh{i}", [128, NOWN * 128], BF16) for i in range(2)]
                    b_qrTh = em.bufs("qrTh", 2)
                    pT = [T(ph, f"pT{i}", [128, 512], BF16) for i in range(3)]
                    b_pT = em.bufs("pT", 3)
                    oah = [T(ph, f"oah{i}", [128, NOWN, 128], BF16) for i in range(2)]
                    b_oah = em.bufs("oah", 2)
                    rs = [T(ph, f"Brs{i}", [128, 1], F32) for i in range(2)]
                    b_rs = em.bufs("Brs", 2)
                    pbld = [P(ph, f"Bpb{i}", [128, 512], F32) for i in range(2)]
                    b_pbld = em.bufs("Bpb", 2)
                    pS = [P(ph, f"BpS{i}", [128, 512], F32) for i in range(2)]
                    b_pS = em.bufs("BpS", 2)
                    pO = [P(ph, f"BpO{i}", [128, 512], F32) for i in range(2)]
                    b_pO = em.bufs("BpO", 2)

                    em.op("pool", lambda e: e.memset(Vh[:, :, 128:132], 1.0), writes=[b_Vones])
                    w_uq_v = w_uq.rearrange("(rc p) c -> p rc c", p=128)
                    w_ukv_v = w_ukv.rearrange("(rc p) c -> p rc c", p=128)

                    def load_head_w(h):
                        i = h % 2
                        em.dma("sp", lambda e: e.dma_start(out=wq_st[i][:], in_=w_uq_v[:, :, h * 192:h * 192 + 128]), writes=[b_wst[i]])
                        em.dma("sp", lambda e: e.dma_start(out=wkv_st[i][:], in_=w_ukv_v[:, :, h * 256:(h + 1) * 256]), writes=[b_wst[i]],
                               group=True)
                        po = (h % 2) * 64
                        em.dma("sp", lambda e: e.dma_start(out=qrTh[i][po:po + 64, :], in_=qrs[po:po + 64, h // 2, :]), writes=[b_qrTh[i]])

                    load_head_w(0)
                    nb = 0
                    ev_i = 0
                    for h in range(NH):
                        i = h % 2
                        po = (h % 2) * 64
                        if h + 1 < NH:
                            load_head_w(h + 1)
                        emit_tab_jobs(4)
                        em.op("dve", lambda e: e.tensor_copy(out=wq_bf[i][:], in_=wq_st[i][:]), reads=[b_wst[i]], writes=[b_wbf[i]])
                        for rc in range(4):
                            em.op("dve", lambda e: e.tensor_scalar(out=wkv_bf[i][:, rc, :], in0=wkv_st[i][:, rc, :], scalar1=gkvB[:, rc:rc + 1],
                                                                   scalar2=None, op0=ALU.mult), reads=[b_wst[i], b_gkvB], writes=[b_wbf[i]], acc=True)
                        for qc in range(4):
                            pb = pbld[nb % 2]
                            b_pb = b_pbld[nb % 2]
                            nb += 1
                            for rc in range(4):
                                em.op("pe", lambda e: e.matmul(pb[:], lhsT=wq_bf[i][:, rc, :], rhs=cqnT[:, rc, qc * 512:(qc + 1) * 512],
                                                               start=(rc == 0), stop=(rc == 3)),
                                      reads=[b_wbf[i]] + b_cqnT[qc * 4:(qc + 1) * 4], writes=[b_pb], acc=(rc > 0))
                            em.op("act", lambda e: e.activation(out=qnT[:, qc * 512:(qc + 1) * 512], in_=pb[:], func=AF.Copy, scale=QSCALE),
                                  reads=[b_pb], writes=[b_qnT[qc]])
                        for tc in range(16):
                            pb = pbld[nb % 2]
                            b_pb = b_pbld[nb % 2]
                            nb += 1
                            for rc in range(4):
                                em.op("pe", lambda e: e.matmul(pb[:], lhsT=wkv_bf[i][:, rc, 0:128], rhs=ckvnT[:, tc * 4:(tc + 1) * 4, rc, :],
                                                               start=(rc == 0), stop=(rc == 3)),
                                      reads=[b_wbf[i]] + b_ckvnT[tc * 4:(tc + 1) * 4], writes=[b_pb], acc=(rc > 0))
                            eng = "act" if (ev_i % 2 == 0) else "dve"
                            ev_i += 1
                            if eng == "act":
                                em.op("act", lambda e: e.activation(out=KnT[:, tc * 512:(tc + 1) * 512], in_=pb[:], func=AF.Copy),
                                      reads=[b_pb], writes=[b_KnT[tc]])
                            else:
                                em.op("dve", lambda e: e.tensor_copy(out=KnT[:, tc * 512:(tc + 1) * 512], in_=pb[:]),
                                      reads=[b_pb], writes=[b_KnT[tc]])
                        for g in range(16):
                            pb = pbld[nb % 2]
                            b_pb = b_pbld[nb % 2]
                            nb += 1
                            for ii in range(4):
                                blk = 4 * g + ii
                                for rc in range(4):
                                    em.op("pe", lambda e: e.matmul(pb[:, ii * 128:(ii + 1) * 128], lhsT=ckvnT[:, blk, rc, :],
                                                                   rhs=wkv_bf[i][:, rc, 128:256], start=(rc == 0), stop=(rc == 3)),
                                          reads=[b_wbf[i], b_ckvnT[blk]], writes=[b_pb], acc=not (ii == 0 and rc == 0))
                            eng = "act" if (ev_i % 2 == 0) else "dve"
                            ev_i += 1
                            src = pb[:].rearrange("p (a d) -> p a d", d=128)
                            if eng == "act":
                                em.op("act", lambda e: e.activation(out=Vh[:, 4 * g:4 * g + 4, 0:128], in_=src, func=AF.Copy),
                                      reads=[b_pb], writes=[b_Vh[g]])
                            else:
                                em.op("dve", lambda e: e.tensor_copy(out=Vh[:, 4 * g:4 * g + 4, 0:128], in_=src),
                                      reads=[b_pb], writes=[b_Vh[g]])

                        work = [(m, c) for m in range(NOWN) for c in range(m + 1)]

                        def emit_S(k):
                            m, c = work[k]
                            ps = pS[k % 2]
                            b_ps = b_pS[k % 2]
                            for ii in range(4):
                                kb = 4 * c + ii
                                em.op("pe", lambda e: e.matmul(ps[:, ii * 128:(ii + 1) * 128], lhsT=KnT[:, kb * 128:(kb + 1) * 128],
                                                               rhs=qnT[:, m * 128:(m + 1) * 128], start=True, stop=False),
                                      reads=[b_KnT[c], b_qnT[m // 4]], writes=[b_ps], acc=(ii > 0))
                                em.op("pe", lambda e: e.matmul(ps[:, ii * 128:(ii + 1) * 128], lhsT=kpeT[po:po + 64, kb * 128:(kb + 1) * 128],
                                                               rhs=qrTh[i][po:po + 64, m * 128:(m + 1) * 128], start=False, stop=True),
                                      reads=[b_kpeT[kb], b_qrTh[i]], writes=[b_ps], acc=True)
                            p = pT[k % 3]
                            b_p = b_pT[k % 3]
                            em.op("act", lambda e: e.activation(out=p[:], in_=ps[:], func=AF.Exp), reads=[b_ps], writes=[b_p])
                            if c == m:
                                em.op("dve", lambda e: e.tensor_tensor(out=p[:], in0=p[:], in1=cm[:], op=ALU.mult), reads=[b_p, b_cm],
                                      writes=[b_p])

                        def emit_PV(k):
                            m, c = work[k]
                            p = pT[k % 3]
                            b_p = b_pT[k % 3]
                            po_ = pO[m % 2]
                            b_po = b_pO[m % 2]
                            for ii in range(4):
                                kb = 4 * c + ii
                                em.op("pe", lambda e: e.matmul(po_[:, 0:129], lhsT=p[:, ii * 128:(ii + 1) * 128], rhs=Vh[:, kb, 0:129],
                                                               start=(c == 0 and ii == 0), stop=(c == m and ii == 3)),
                                      reads=[b_p, b_Vh[c], b_Vones], writes=[b_po], acc=not (c == 0 and ii == 0))
                            if c == m:
                                r = rs[m % 2]
                                b_r = b_rs[m % 2]
                                em.op("dve", lambda e: e.reciprocal(out=r[:], in_=po_[:, 128:129]), reads=[b_po], writes=[b_r])
                                em.op("dve", lambda e: e.tensor_scalar(out=oah[i][:, m, :], in0=po_[:, 0:128], scalar1=r[:, 0:1], scalar2=None,
                                                                       op0=ALU.mult), reads=[b_po, b_r], writes=[b_oah[i]], acc=(m > 0))

                        emit_S(0)
                        for k in range(len(work)):
                            if k + 1 < len(work):
                                emit_S(k + 1)
                            emit_PV(k)
                        em.dma("pool", lambda e: e.dma_start(out=oas[:, h * 128:(h + 1) * 128].rearrange("(m p) d -> p m d", p=128),
                                                             in_=oah[i][:]), reads=[b_oah[i]])
                    em.barrier()
        if stop == "B":
            em.barrier()
            return nc

        with ExitStack() as ph:
            lng, b_lng = load_small(ph, "lng", lng_d[:, :], [128, 2048])
            lnb, b_lnb = load_small(ph, "lnb", lnb_d[:, :], [128, 2048])
            bsp, b_bsp = load_small(ph, "bsp", bsp_d[:, :], [128, 8])
            wspf, b_wspf = load_small(ph, "wspf", wspT_d[:, :, :], [128, 8, 128])
            trif, b_trif = load_small(ph, "trif", triT_d[:, :], [128, 128])
            wsT = T(ph, "wsT", [128, 8, 128], BF16)
            b_wsT = em.buf("wsT")
            em.op("dve", lambda e: e.tensor_tensor(out=wsT[:], in0=wspf[:], in1=trif[:].unsqueeze(1).to_broadcast([128, 8, 128]),
                                                   op=ALU.mult), reads=[b_wspf, b_trif], writes=[b_wsT])
            wo = T(ph, "wo", [128, 16, 2048], BF16)
            b_wo = em.bufs("wo", 4)
            with ExitStack() as tmp:
                wst = T(tmp, "Cwst", [128, 16, 256], F32)
                b_wst = em.buf("Cwst")
                for dc8 in range(8):
                    em.dma("sp", lambda e: e.dma_start(out=wst[:], in_=w_out[:, dc8 * 256:(dc8 + 1) * 256].rearrange("(kc p) n -> p kc n", p=128)),
                           writes=[b_wst])
                    for hh in range(2):
                        if hh == 0:
                            em.op("dve", lambda e: e.tensor_copy(out=wo[:, hh * 8:(hh + 1) * 8, dc8 * 256:(dc8 + 1) * 256],
                                                                 in_=wst[:, hh * 8:(hh + 1) * 8, :]), reads=[b_wst], writes=[b_wo[dc8 // 2]], acc=True)
                        else:
                            em.op("act", lambda e: e.activation(out=wo[:, hh * 8:(hh + 1) * 8, dc8 * 256:(dc8 + 1) * 256],
                                                                in_=wst[:, hh * 8:(hh + 1) * 8, :], func=AF.Copy), reads=[b_wst], writes=[b_wo[dc8 // 2]], acc=True)
                em.barrier()
            ld = {}
            for nm in ["u", "v", "gA", "gB", "oa"]:
                ld[nm] = ([T(ph, f"C{nm}{i}", [128, 2048], BF16) for i in range(2)], em.bufs(f"C{nm}", 2))
            xt = [T(ph, f"Cx{i}", [128, 2048], F32) for i in range(2)]
            b_xt = em.bufs("Cx", 2)
            junk = T(ph, "Cjunk", [128, 2048], BF16)
            b_junk = em.buf("Cjunk")
            stt = T(ph, "Cst", [128, 16], F32)
            b_stt = em.buf("Cst")
            tf = T(ph, "Ctf", [128, 2048], F32)
            b_tf = em.buf("Ctf")
            vn = T(ph, "Cvn", [128, 2048], BF16)
            b_vn = em.buf("Cvn")
            ob = tf
            b_ob = b_tf
            t1 = T(ph, "Ct1", [128, 2048], BF16)
            b_t1 = em.buf("Ct1")
            mg = T(ph, "Cmg", [128, 2048], BF16)
            b_mg = em.buf("Cmg")
            mT = T(ph, "CmT", [128, 16, 128], BF16)
            b_mT = em.buf("CmT")
            x1 = [T(ph, "Cxone", [128, 2048], F32)] * 2
            b_x1 = [em.buf("Cxone")] * 2
            pbig = P(ph, "Cpbig", [128, 2048], F32)
            b_pbig = em.buf("Cpbig")
            tpp = P(ph, "Ctp", [128, 16, 128], BF16)
            b_tpp = em.buf("Ctp")

            pmix = P(ph, "Cpmix", [128, 1024], F32)
            b_pmix = em.buf("Cpmix")
            mgs = [mg, T(ph, "Cmg1", [128, 2048], BF16)]
            b_mgs = [b_mg, em.buf("Cmg1")]

            def c1_loads(m):
                i = m % 2
                r0 = m * 128
                em.dma("sp", lambda e: e.dma_start(out=ld["u"][0][i][:], in_=zs[r0:r0 + 128, 0:2048]), writes=[ld["u"][1][i]])
                em.dma("sp", lambda e: e.dma_start(out=ld["v"][0][i][:], in_=zs[r0:r0 + 128, 2048:4096]), writes=[ld["v"][1][i]])
                em.dma("sp", lambda e: e.dma_start(out=ld["gA"][0][i][:], in_=gs[r0:r0 + 128, 0:2048]), writes=[ld["gA"][1][i]])
                em.dma("sp", lambda e: e.dma_start(out=ld["gB"][0][i][:], in_=gs[r0:r0 + 128, 2048:4096]), writes=[ld["gB"][1][i]])
                em.dma("sp", lambda e: e.dma_start(out=ld["oa"][0][i][:], in_=oas[r0:r0 + 128, :]), writes=[ld["oa"][1][i]])

            def C1_S1(m):
                i = m % 2
                if m + 1 < NOWN:
                    c1_loads(m + 1)
                u, b_u = ld["u"][0][i], ld["u"][1][i]
                v, b_v = ld["v"][0][i], ld["v"][1][i]
                gA, b_gA = ld["gA"][0][i], ld["gA"][1][i]
                gB, b_gB = ld["gB"][0][i], ld["gB"][1][i]
                oa, b_oa = ld["oa"][0][i], ld["oa"][1][i]
                mgc, b_mgc = mgs[i], b_mgs[i]
                em.op("act", lambda e: e.activation(out=junk[:], in_=v[:], func=AF.Copy, accum_out=stt[:, 0:1]), reads=[b_v], writes=[b_junk, b_stt])
                em.op("act", lambda e: e.activation(out=junk[:], in_=v[:], func=AF.Square, accum_out=stt[:, 1:2]), reads=[b_v], writes=[b_junk, b_stt],
                      acc=True)
                em.op("dve", lambda e: e.tensor_scalar(out=stt[:, 2:4], in0=stt[:, 0:2], scalar1=1.0 / 2048, scalar2=None, op0=ALU.mult),
                      reads=[b_stt], writes=[b_stt])
                em.op("dve", lambda e: e.tensor_tensor(out=stt[:, 4:5], in0=stt[:, 2:3], in1=stt[:, 2:3], op=ALU.mult),
                      reads=[b_stt], writes=[b_stt])
                em.op("dve", lambda e: e.tensor_tensor(out=stt[:, 5:6], in0=stt[:, 3:4], in1=stt[:, 4:5], op=ALU.subtract),
                      reads=[b_stt], writes=[b_stt])
                em.op("dve", lambda e: e.tensor_scalar(out=stt[:, 6:7], in0=stt[:, 5:6], scalar1=EPS, scalar2=None, op0=ALU.add),
                      reads=[b_stt], writes=[b_stt])
                em.op("act", lambda e: e.activation(out=stt[:, 7:8], in_=stt[:, 6:7], func=AF.Sqrt), reads=[b_stt], writes=[b_stt])
                em.op("dve", lambda e: e.reciprocal(out=stt[:, 8:9], in_=stt[:, 7:8]), reads=[b_stt], writes=[b_stt])
                em.op("dve", lambda e: e.tensor_scalar(out=tf[:], in0=v[:], scalar1=stt[:, 2:3], scalar2=stt[:, 8:9], op0=ALU.subtract,
                                                       op1=ALU.mult), reads=[b_v, b_stt], writes=[b_tf])
                em.op("dve", lambda e: e.tensor_tensor(out=tf[:], in0=tf[:], in1=lng[:], op=ALU.mult), reads=[b_tf, b_lng], writes=[b_tf])
                em.op("dve", lambda e: e.tensor_tensor(out=vn[:], in0=tf[:], in1=lnb[:], op=ALU.add), reads=[b_tf, b_lnb], writes=[b_vn])

            def C1_S1b(m):
                i = m % 2
                u, b_u = ld["u"][0][i], ld["u"][1][i]
                gA, b_gA = ld["gA"][0][i], ld["gA"][1][i]
                gB, b_gB = ld["gB"][0][i], ld["gB"][1][i]
                oa, b_oa = ld["oa"][0][i], ld["oa"][1][i]
                mgc, b_mgc = mgs[i], b_mgs[i]
                for half in range(2):
                    for gg in range(4):
                        g = half * 4 + gg
                        em.op("pe", lambda e: e.matmul(pmix[:, gg * 256:(gg + 1) * 256], lhsT=wsT[:, g, :], rhs=vn[:, g * 256:(g + 1) * 256],
                                                       start=True, stop=True), reads=[b_wsT, b_vn], writes=[b_pmix], acc=(gg > 0))
                    for gg in range(4):
                        g = half * 4 + gg
                        em.op("dve", lambda e: e.scalar_tensor_tensor(out=ob[:, g * 256:(g + 1) * 256], in0=pmix[:, gg * 256:(gg + 1) * 256],
                                                                       scalar=bsp[:, g:g + 1], in1=u[:, g * 256:(g + 1) * 256], op0=ALU.add,
                                                                       op1=ALU.mult), reads=[b_pmix, b_bsp, b_u], writes=[b_ob], acc=(g > 0))
                em.op("dve", lambda e: e.tensor_tensor(out=t1[:], in0=gA[:], in1=oa[:], op=ALU.mult), reads=[b_gA, b_oa], writes=[b_t1])
                em.op("dve", lambda e: e.tensor_tensor(out=ob[:], in0=ob[:], in1=gB[:], op=ALU.mult), reads=[b_ob, b_gB], writes=[b_ob])
                em.op("dve", lambda e: e.tensor_tensor(out=mgc[:], in0=ob[:], in1=t1[:], op=ALU.add), reads=[b_ob, b_t1], writes=[b_mgc])

            def C1_S2(m):
                i = m % 2
                mgc, b_mgc = mgs[i], b_mgs[i]
                em.dma("sp", lambda e: e.dma_start(out=xt[i][:], in_=xo[m * 128:(m + 1) * 128, :]), writes=[b_xt[i]])
                for kc in range(16):
                    em.op("pe", lambda e: e.transpose(out=tpp[:, kc, :], in_=mgc[:, kc * 128:(kc + 1) * 128], identity=identb[:]),
                          reads=[b_mgc, b_identb], writes=[b_tpp], acc=(kc > 0))
                em.op("act", lambda e: e.activation(out=mT[:], in_=tpp[:], func=AF.Copy), reads=[b_tpp], writes=[b_mT])
                for dc in range(4):
                    for kc in range(16):
                        em.op("pe", lambda e: e.matmul(pbig[:, dc * 512:(dc + 1) * 512], lhsT=mT[:, kc, :], rhs=wo[:, kc, dc * 512:(dc + 1) * 512],
                                                       start=(kc == 0), stop=(kc == 15)), reads=[b_mT, b_wo[dc]], writes=[b_pbig],
                              acc=not (dc == 0 and kc == 0))
                em.op("dve", lambda e: e.tensor_tensor(out=x1[i][:], in0=pbig[:], in1=xt[i][:], op=ALU.add), reads=[b_pbig, b_xt[i]],
                      writes=[b_x1[i]])
                em.dma("pool", lambda e: e.dma_start(out=x1s[m * 128:(m + 1) * 128, :], in_=x1[i][:]), reads=[b_x1[i]])

            c1_loads(0)
            C1_S1(0)
            C1_S1b(0)
            for m in range(NOWN):
                if m + 1 < NOWN:
                    C1_S1(m + 1)
                C1_S2(m)
                if m + 1 < NOWN:
                    C1_S1b(m + 1)
            em.barrier()
        if stop == "C1":
            em.barrier()
            return nc

        emit_tab_jobs(64)
        with ExitStack() as ph:
            g2b, b_g2b = load_small(ph, "g2b", g2_d[:, :], [128, 2048])
            gfb, b_gfb = load_small(ph, "gfb", gf_d[:, :], [128, 2048])
            iota16, b_iota4 = load_small(ph, "iota16", iota4_d[:, 0:16], [128, 16])
            kT = T(ph, "kT", [128, 16, 128], BF16)
            b_kT = em.buf("kT")
            wpq = T(ph, "wpq", [128, 16, 2048], BF16)
            b_wpq = em.bufs("wpq", 4)
            with ExitStack() as tmp:
                kTf, b_kTf = load_small(tmp, "kTf", keysT_d[:, :, :], [128, 16, 128])
                em.op("dve", lambda e: e.tensor_copy(out=kT[:], in_=kTf[:]), reads=[b_kTf], writes=[b_kT])
                wst = T(tmp, "Dwst", [128, 16, 512], F32)
                b_wst = em.buf("Dwst")
                for dc in range(4):
                    em.dma("sp", lambda e: e.dma_start(out=wst[:], in_=w_pq[:, dc * 512:(dc + 1) * 512].rearrange("(kc p) n -> p kc n", p=128)),
                           writes=[b_wst])
                    em.op("dve", lambda e: e.tensor_copy(out=wpq[:, :, dc * 512:(dc + 1) * 512], in_=wst[:]), reads=[b_wst], writes=[b_wpq[dc]])
                em.barrier()
            NGU = 6
            NGV = 6
            ug = [T(ph, f"ug{i}", [128, 2048], BF16) for i in range(NGU)]
            b_ug = em.bufs("ug", NGU)
            vg = [T(ph, f"vg{i}", [128, 2048], BF16) for i in range(NGV)]
            b_vg = em.bufs("vg", NGV)
            xP = T(ph, "DxP", [128, 2048], F32)
            b_xP = em.buf("DxP")
            xE = T(ph, "DxE", [128, 2048], F32)
            b_xE = em.buf("DxE")
            stt = T(ph, "Dst", [128, 16], F32)
            b_stt = em.buf("Dst")
            stE = T(ph, "DstE", [128, 16], F32)
            b_stE = em.buf("DstE")
            h2 = [T(ph, f"Dh2_{i}", [128, 2048], BF16) for i in range(2)]
            b_h2 = em.bufs("Dh2_", 2)
            h2T = T(ph, "Dh2T", [128, 16, 128], BF16)
            b_h2T = em.buf("Dh2T")
            qb = T(ph, "Dqb", [128, 2048], BF16)
            b_qb = em.buf("Dqb")
            qT = h2T
            b_qT = b_h2T
            scr = T(ph, "Dscr", [128, 2048], F32)
            b_scr = em.buf("Dscr")
            sc = scr[:].rearrange("p (a n) -> p a n", n=128)
            b_sc = b_scr
            cand = scr[:].rearrange("p (h c) -> p h c", c=256)
            b_cand = b_scr
            eq = scr
            b_eq = b_scr
            scrE = T(ph, "DscrE", [128, 1024], BF16)
            b_scrE = em.buf("DscrE")
            wk = T(ph, "Dwk", [128, 256], F32)
            b_wk = em.buf("Dwk")
            t16 = T(ph, "Dt16", [128, 16, 16], F32)
            b_t16 = em.buf("Dt16")
            i16 = T(ph, "Di16", [128, 16, 16], U32)
            b_i16 = em.buf("Di16")
            i16f = T(ph, "Di16f", [128, 16, 16], F32)
            b_i16f = em.buf("Di16f")
            ts = T(ph, "Dts", [128, 8, 16], F32)
            b_ts = em.buf("Dts")
            pidx = T(ph, "Dpidx", [128, 8, 16], U32)
            b_pidx = em.buf("Dpidx")
            pa_i = T(ph, "Dpai", [128, 8, 16], U32)
            pb_i = T(ph, "Dpbi", [128, 8, 16], U32)
            pa_f = T(ph, "Dpaf", [128, 8, 16], F32)
            pb_f = T(ph, "Dpbf", [128, 8, 16], F32)
            b_pab = em.buf("Dpab")
            isel = T(ph, "Disel", [128, 2, 128], F32)
            b_isel = em.buf("Disel")
            idxf = T(ph, "Didxf", [128, 128], F32)
            b_idxf = em.buf("Didxf")
            ex = T(ph, "Dex", [128, 8, 16], F32)
            b_ex = em.buf("Dex")
            zz = T(ph, "Dzz", [128, 16], F32)
            b_zz = em.buf("Dzz")
            gate = T(ph, "Dgate", [128, 128], F32)
            b_gate = em.buf("Dgate")
            idxT = [T(ph, f"DidxT{i}", [128, 128], I32) for i in range(3)]
            b_idxT = em.bufs("DidxT", 3)
            gateT = [T(ph, f"DgateT{i}", [128, 128], F32) for i in range(3)]
            b_gateT = em.bufs("DgateT", 3)
            hu4 = T(ph, "Dhu4", [128, 4, 128], F32)
            b_hu4 = em.buf("Dhu4")
            hu = T(ph, "Dhu", [128, 128], F32)
            b_hu = em.buf("Dhu")
            Aa = [T(ph, f"DA{i}", [128, 128], F32) for i in range(2)]
            b_Aa = em.bufs("DA", 2)
            AD = [T(ph, f"DAD{i}", [128, 32 * 128], BF16) for i in range(2)]
            b_AD = em.bufs("DAD", 2)
            pA = P(ph, "DpA", [128, 2048], F32)
            b_pA = em.buf("DpA")
            bq = [P(ph, f"Dbq{i}", [128, 1024], F32) for i in range(2)]
            b_bq = em.bufs("Dbq", 2)
            pP = [bq[0][:, 0:512], bq[1][:, 0:512]]
            b_pP = b_bq
            pPb = [pP[k].bitcast(BF16).rearrange("p (k t) -> p k t", t=128) for k in range(2)]

            em.op("dve", lambda e: e.memset(AD[0][:], 0.0), writes=[b_AD[0]])
            em.op("dve", lambda e: e.memset(AD[1][:], 0.0), writes=[b_AD[1]])
            cnt = {"pp": 0, "bq": 0, "gu": 0, "gv": 0}

            def next_pp():
                k = cnt["pp"] % 2
                cnt["pp"] += 1
                return k

            def stage_P_gen(m):
                xx = xP
                b_xx = b_xP
                hh2 = h2[m % 2]
                b_hh2 = b_h2[m % 2]
                em.dma("sp", lambda e: e.dma_start(out=xx[:], in_=x1s[m * 128:(m + 1) * 128, :]), writes=[b_xx])
                em.op("dve", lambda e: e.memset(stt[:], 0.0), writes=[b_stt])
                em.op("dve", lambda e: e.scalar_tensor_tensor(out=scr[:], in0=xx[:], scalar=1.0, in1=xx[:], op0=ALU.mult, op1=ALU.mult,
                                                               accum_out=stt[:, 0:1]), reads=[b_xx], writes=[b_scr, b_stt])
                cr = rstd_chain(stt, b_stt, 0, 1, 2048)
                em.op("dve", lambda e: e.scalar_tensor_tensor(out=hh2[:], in0=xx[:], scalar=stt[:, cr:cr + 1], in1=g2b[:], op0=ALU.mult,
                                                               op1=ALU.mult), reads=[b_xx, b_stt, b_g2b], writes=[b_hh2])
                for half in range(2):
                    yield
                    k = next_pp()
                    for kk in range(8):
                        kc = half * 8 + kk
                        em.op("pe", lambda e: e.transpose(out=pPb[k][:, kk, :], in_=hh2[:, kc * 128:(kc + 1) * 128], identity=identb[:]),
                              reads=[b_hh2, b_identb], writes=[b_pP[k]], acc=(kk > 0))
                    em.op("act", lambda e: e.activation(out=h2T[:, half * 8:(half + 1) * 8, :], in_=pPb[k], func=AF.Copy),
                          reads=[b_pP[k]], writes=[b_h2T], acc=(half > 0))
                for dc in range(4):
                    yield
                    k = next_pp()
                    for kc in range(16):
                        em.op("pe", lambda e: e.matmul(pP[k][:], lhsT=h2T[:, kc, :], rhs=wpq[:, kc, dc * 512:(dc + 1) * 512],
                                                       start=(kc == 0), stop=(kc == 15)), reads=[b_h2T, b_wpq[dc]], writes=[b_pP[k]],
                              acc=(kc > 0))
                    em.op("act", lambda e: e.activation(out=qb[:, dc * 512:(dc + 1) * 512], in_=pP[k][:], func=AF.Copy),
                          reads=[b_pP[k]], writes=[b_qb], acc=(dc > 0))
                for half in range(2):
                    yield
                    k = next_pp()
                    for kk in range(8):
                        hp = half * 8 + kk
                        em.op("pe", lambda e: e.transpose(out=pPb[k][:, kk, :], in_=qb[:, hp * 128:(hp + 1) * 128], identity=identb[:]),
                              reads=[b_qb, b_identb], writes=[b_pP[k]], acc=(kk > 0))
                    em.op("act", lambda e: e.activation(out=qT[:, half * 8:(half + 1) * 8, :], in_=pPb[k], func=AF.Copy),
                          reads=[b_pP[k]], writes=[b_qT], acc=(half > 0))
                for qd in range(4):
                    yield
                    k = next_pp()
                    for ii in range(4):
                        hp = qd * 4 + ii
                        em.op("pe", lambda e: e.matmul(pP[k][:, ii * 128:(ii + 1) * 128], lhsT=qT[:, hp, :], rhs=kT[:, hp, :], start=True, stop=True),
                              reads=[b_qT, b_kT], writes=[b_pP[k]], acc=(ii > 0))
                    em.op("act", lambda e: e.activation(out=scr[:, qd * 512:(qd + 1) * 512], in_=pP[k][:], func=AF.Copy),
                          reads=[b_pP[k]], writes=[b_sc], acc=(qd > 0))
                for hp in range(16):
                    yield
                    em.op("dve", lambda e: e.max(out=t16[:, hp, 0:8], in_=sc[:, hp, :]), reads=[b_sc], writes=[b_t16], acc=True)
                    em.op("dve", lambda e: e.max_index(out=i16[:, hp, 0:8], in_max=t16[:, hp, 0:8], in_values=sc[:, hp, :]),
                          reads=[b_sc, b_t16], writes=[b_i16], acc=True)
                    em.op("dve", lambda e: e.match_replace(out=wk[:, 0:128], in_to_replace=t16[:, hp, 0:8], in_values=sc[:, hp, :],
                                                           imm_value=-1e30), reads=[b_sc, b_t16], writes=[b_wk])
                    em.op("dve", lambda e: e.max(out=t16[:, hp, 8:16], in_=wk[:, 0:128]), reads=[b_wk], writes=[b_t16], acc=True)
                    em.op("dve", lambda e: e.max_index(out=i16[:, hp, 8:16], in_max=t16[:, hp, 8:16], in_values=wk[:, 0:128]),
                          reads=[b_wk, b_t16], writes=[b_i16], acc=True)
                yield
                em.op("dve", lambda e: e.tensor_copy(out=i16f[:], in_=i16[:]), reads=[b_i16], writes=[b_i16f])
                t16v = t16[:].rearrange("p (h two) k -> p h two k", two=2)
                i16v = i16f[:].rearrange("p (h two) k -> p h two k", two=2)
                cand4 = cand.rearrange("p h (a b) -> p h a b", b=16)
                em.op("dve", lambda e: e.tensor_tensor(out=cand4, in0=t16v[:, :, 0, :].unsqueeze(3).to_broadcast([128, 8, 16, 16]),
                                                       in1=t16v[:, :, 1, :].unsqueeze(2).to_broadcast([128, 8, 16, 16]), op=ALU.add),
                      reads=[b_t16], writes=[b_cand])
                for hh in range(8):
                    yield
                    em.op("dve", lambda e: e.max(out=ts[:, hh, 0:8], in_=cand[:, hh, :]), reads=[b_cand], writes=[b_ts], acc=True)
                    em.op("dve", lambda e: e.max_index(out=pidx[:, hh, 0:8], in_max=ts[:, hh, 0:8], in_values=cand[:, hh, :]),
                          reads=[b_cand, b_ts], writes=[b_pidx], acc=True)
                    em.op("dve", lambda e: e.match_replace(out=wk[:], in_to_replace=ts[:, hh, 0:8], in_values=cand[:, hh, :],
                                                           imm_value=-1e30), reads=[b_cand, b_ts], writes=[b_wk])
                    em.op("dve", lambda e: e.max(out=ts[:, hh, 8:16], in_=wk[:]), reads=[b_wk], writes=[b_ts], acc=True)
                    em.op("dve", lambda e: e.max_index(out=pidx[:, hh, 8:16], in_max=ts[:, hh, 8:16], in_values=wk[:]),
                          reads=[b_wk, b_ts], writes=[b_pidx], acc=True)
                yield
                em.op("dve", lambda e: e.tensor_tensor(out=ex[:], in0=ts[:], in1=ts[:, :, 0:1].to_broadcast([128, 8, 16]), op=ALU.subtract),
                      reads=[b_ts], writes=[b_ex])
                em.op("act", lambda e: e.activation(out=ex[:], in_=ex[:], func=AF.Exp), reads=[b_ex], writes=[b_ex])
                em.op("dve", lambda e: e.tensor_reduce(out=zz[:, 0:8], in_=ex[:], axis=AX.X, op=ALU.add), reads=[b_ex], writes=[b_zz])
                em.op("dve", lambda e: e.reciprocal(out=zz[:, 8:16], in_=zz[:, 0:8]), reads=[b_zz], writes=[b_zz])
                em.op("dve", lambda e: e.tensor_tensor(out=gate[:].rearrange("p (h k) -> p h k", k=16), in0=ex[:],
                                                       in1=zz[:, 8:16].unsqueeze(2).to_broadcast([128, 8, 16]), op=ALU.mult),
                      reads=[b_ex, b_zz], writes=[b_gate])
                em.op("dve", lambda e: e.tensor_scalar(out=pa_i[:], in0=pidx[:], scalar1=4, scalar2=None, op0=ALU.logical_shift_right),
                      reads=[b_pidx], writes=[b_pab])
                em.op("dve", lambda e: e.tensor_scalar(out=pb_i[:], in0=pidx[:], scalar1=15, scalar2=None, op0=ALU.bitwise_and),
                      reads=[b_pidx], writes=[b_pab], acc=True)
                em.op("dve", lambda e: e.tensor_copy(out=pa_f[:], in_=pa_i[:]), reads=[b_pab], writes=[b_pab], acc=True)
                em.op("dve", lambda e: e.tensor_copy(out=pb_f[:], in_=pb_i[:]), reads=[b_pab], writes=[b_pab], acc=True)
                eq4 = eq[:].rearrange("p (h k a) -> p h k a", h=8, k=16)
                io4 = iota16[:].unsqueeze(1).unsqueeze(1).to_broadcast([128, 8, 16, 16])
                for half, pf in enumerate([pa_f, pb_f]):
                    yield
                    em.op("dve", lambda e: e.tensor_tensor(out=eq4, in0=pf[:].unsqueeze(3).to_broadcast([128, 8, 16, 16]), in1=io4,
                                                           op=ALU.is_equal), reads=[b_pab, b_iota4], writes=[b_eq])
                    em.op("dve", lambda e: e.tensor_tensor(out=eq4, in0=eq4, in1=i16v[:, :, half, :].unsqueeze(2).to_broadcast([128, 8, 16, 16]),
                                                           op=ALU.mult), reads=[b_eq, b_i16f], writes=[b_eq])
                    em.op("dve", lambda e: e.tensor_reduce(out=isel[:, half, :], in_=eq[:].rearrange("p (r a) -> p r a", a=16), axis=AX.X,
                                                           op=ALU.add), reads=[b_eq], writes=[b_isel], acc=(half > 0))
                em.op("dve", lambda e: e.scalar_tensor_tensor(out=idxf[:], in0=isel[:, 0, :], scalar=128.0, in1=isel[:, 1, :], op0=ALU.mult,
                                                               op1=ALU.add), reads=[b_isel], writes=[b_idxf])
                yield
                k = next_pp()
                em.op("pe", lambda e: e.transpose(out=pP[k][:, 0:128], in_=idxf[:], identity=identf[:]), reads=[b_idxf, b_identf],
                      writes=[b_pP[k]])
                em.op("pe", lambda e: e.transpose(out=pP[k][:, 128:256], in_=gate[:], identity=identf[:]), reads=[b_gate, b_identf],
                      writes=[b_pP[k]], acc=True)
                em.op("dve", lambda e: e.tensor_copy(out=idxT[m % 3][:], in_=pP[k][:, 0:128]), reads=[b_pP[k]], writes=[b_idxT[m % 3]])
                em.op("dve", lambda e: e.tensor_copy(out=gateT[m % 3][:], in_=pP[k][:, 128:256]), reads=[b_pP[k]], writes=[b_gateT[m % 3]])

            def stage_P(m):
                for _ in stage_P_gen(m):
                    pass

            def U_gather(m, t):
                k = cnt["gu"] % NGU
                cnt["gu"] += 1
                em.dma("pool", lambda e: e.indirect_dma_start(out=ug[k][:], out_offset=None, in_=ub[:, :],
                                                              in_offset=bass.IndirectOffsetOnAxis(ap=idxT[m % 3][:, t:t + 1], axis=0)),
                       reads=[b_idxT[m % 3], b_ub], writes=[b_ug[k]])
                return k

            def U_part(m, t, k, half):
                kb = cnt["bq"] % 2
                cnt["bq"] += 1
                for c2 in range(2):
                    col = half * 1024 + c2 * 512
                    em.op("pe", lambda e: e.matmul(bq[kb][:, c2 * 512:(c2 + 1) * 512], lhsT=identb[:, t:t + 1].to_broadcast([128, 128]),
                                                   rhs=h2[m % 2][:, col:col + 512], start=True, stop=True),
                          reads=[b_identb, b_h2[m % 2]], writes=[b_bq[kb]], acc=(c2 > 0))
                em.op("dve", lambda e: e.scalar_tensor_tensor(out=scrE[:], in0=ug[k][:, half * 1024:(half + 1) * 1024], scalar=1.0,
                                                               in1=bq[kb][:], op0=ALU.mult, op1=ALU.mult, accum_out=hu4[:, half, t:t + 1]),
                      reads=[b_ug[k], b_bq[kb]], writes=[b_scrE, b_hu4], acc=True)

            def U_tok(m, t):
                k = U_gather(m, t)
                for c in range(2):
                    U_part(m, t, k, c)

            def U_start(m):
                em.op("dve", lambda e: e.memset(hu4[:], 0.0), writes=[b_hu4])

            def U_finish(m):
                em.op("dve", lambda e: e.tensor_tensor(out=hu[:], in0=hu4[:, 0, :], in1=hu4[:, 1, :], op=ALU.add), reads=[b_hu4], writes=[b_hu])
                em.op("act", lambda e: e.activation(out=hu[:], in_=hu[:], func=AF.Gelu), reads=[b_hu], writes=[b_hu])
                em.op("dve", lambda e: e.tensor_tensor(out=Aa[m % 2][:], in0=hu[:], in1=gateT[m % 3][:], op=ALU.mult),
                      reads=[b_hu, b_gateT[m % 3]], writes=[b_Aa[m % 2]])

            def V_gather(m, t):
                sb = t // 32
                n = 4 * m + sb
                ad = AD[n % 2]
                b_ad = b_AD[n % 2]
                if t % 32 == 0:
                    if n >= 2:
                        sbo = (n - 2) % 4
                        em.op("dve", lambda e: e.memset(ad[:, 32 * sbo:32 * sbo + 129 * 31 + 1:129], 0.0), writes=[b_ad])
                    em.op("dve", lambda e: e.tensor_copy(out=ad[:, 32 * sb:32 * sb + 129 * 31 + 1:129], in_=Aa[m % 2][:, sb * 32:(sb + 1) * 32]),
                          reads=[b_Aa[m % 2]], writes=[b_ad])
                k = cnt["gv"] % NGV
                cnt["gv"] += 1
                em.dma("pool", lambda e: e.indirect_dma_start(out=vg[k][:], out_offset=None, in_=vb[:, :],
                                                              in_offset=bass.IndirectOffsetOnAxis(ap=idxT[m % 3][:, t:t + 1], axis=0)),
                       reads=[b_idxT[m % 3], b_vb], writes=[b_vg[k]])
                return k

            def V_part(m, t, k, c):
                n = 4 * m + t // 32
                ad = AD[n % 2]
                b_ad = b_AD[n % 2]
                tl = t % 32
                em.op("pe", lambda e: e.matmul(pA[:, c * 512:(c + 1) * 512], lhsT=ad[:, tl * 128:(tl + 1) * 128],
                                               rhs=vg[k][:, c * 512:(c + 1) * 512], start=(t == 0), stop=(t == 127)),
                      reads=[b_ad, b_vg[k]], writes=[b_pA], acc=not (t == 0 and c == 0))

            def V_prefetch(m):
                em.dma("sp", lambda e: e.dma_start(out=xE[:], in_=x1s[m * 128:(m + 1) * 128, :]), writes=[b_xE])

            def V_epilogue(m):
                xx = xE
                b_xx = b_xE
                em.op("dve", lambda e: e.tensor_tensor(out=xx[:], in0=pA[:], in1=xx[:], op=ALU.add), reads=[b_pA, b_xx], writes=[b_xx])
                em.op("dve", lambda e: e.memset(stE[:], 0.0), writes=[b_stE])
                em.op("dve", lambda e: e.scalar_tensor_tensor(out=qb[:], in0=xx[:], scalar=1.0, in1=xx[:], op0=ALU.mult, op1=ALU.mult,
                                                               accum_out=stE[:, 8:9]), reads=[b_xx], writes=[b_qb, b_stE])
                cr = rstd_chain(stE, b_stE, 8, 9, 2048)
                em.op("dve", lambda e: e.scalar_tensor_tensor(out=xx[:], in0=xx[:], scalar=stE[:, cr:cr + 1], in1=gfb[:], op0=ALU.mult,
                                                               op1=ALU.mult), reads=[b_xx, b_stE, b_gfb], writes=[b_xx])
                em.dma("sp", lambda e: e.dma_start(out=y[m * 128:(m + 1) * 128, :], in_=xx[:]), reads=[b_xx])

            stage_P(0)
            stage_P(1)
            U_start(0)
            for t in range(128):
                U_tok(0, t)
            U_finish(0)
            for m in range(NOWN):
                pgen = stage_P_gen(m + 2) if m + 2 < NOWN else None
                if m + 1 < NOWN:
                    U_start(m + 1)
                V_prefetch(m)
                for t in range(128):
                    if pgen is not None and t >= 4 and t % 2 == 0:
                        if next(pgen, "done") == "done":
                            pgen = None
                    ku = U_gather(m + 1, t) if m + 1 < NOWN else None
                    kv = V_gather(m, t)
                    for c in range(4):
                        if ku is not None and c % 2 == 0:
                            U_part(m + 1, t, ku, c // 2)
                        V_part(m, t, kv, c)
                if pgen is not None:
                    for _ in pgen:
                        pass
                if m + 1 < NOWN:
                    U_finish(m + 1)
                V_epilogue(m)
            em.barrier()
        print("instructions:", em.ninst, "semaphores:", em.nsem)
    return nc


def _prep_inputs(x, positions, g_norm1, w_in, g_q_a, w_uq, g_kv_a, w_ukv, sgu_ln_g, sgu_ln_b, w_spatial, b_spatial,
                 b_gate, w_out, g_norm2, w_peer_q, peer_keys, peer_u, peer_v, g_final):
    f = lambda a: np.ascontiguousarray(np.asarray(a, dtype=np.float32))
    x = f(x)
    positions = np.asarray(positions).astype(np.int32)
    rep = lambda v: np.ascontiguousarray(np.broadcast_to(f(v).reshape(1, -1), (128, f(v).size)))
    pk = lambda v, n: np.ascontiguousarray(f(v).reshape(n, 128).T)
    freqs = (10000.0 ** (-np.arange(0, 64, 2, dtype=np.float32) / np.float32(64))).astype(np.float32)
    iota4 = np.ascontiguousarray(np.broadcast_to(np.tile(np.arange(16, dtype=np.float32), 128).reshape(1, 2048), (128, 2048)))
    kk = np.arange(128)
    tri = (kk[:, None] <= kk[None, :]).astype(np.float32)
    shared = {
        "freqs": rep(freqs),
        "ident": np.eye(128, dtype=np.float32),
        "iota4": iota4,
        "g1": pk(g_norm1[0], 16),
        "w_in": f(w_in[0]),
        "gq": pk(g_q_a[0], 4),
        "gkv": pk(g_kv_a[0], 4),
        "w_uq": f(w_uq[0]).reshape(512, 3072),
        "w_ukv": f(w_ukv[0]).reshape(512, 4096),
        "lng": rep(sgu_ln_g[0]),
        "lnb": rep(sgu_ln_b[0]),
        "wspT": np.ascontiguousarray(f(w_spatial[0]).transpose(2, 0, 1)),
        "triT": tri,
        "bsp": np.ascontiguousarray(f(b_spatial[0]).T),
        "bgate": rep(b_gate[0]),
        "w_out": f(w_out[0]),
        "g2b": rep(g_norm2[0]),
        "w_pq": f(w_peer_q[0]).reshape(2048, 2048),
        "keysT": np.ascontiguousarray(f(peer_keys[0]).reshape(16, 128, 128).transpose(2, 0, 1)),
        "peer_u": f(peer_u[0]),
        "peer_v": f(peer_v[0]),
        "gfb": rep(g_final),
    }
    in_maps = []
    for c in range(8):
        b, j = c // 4, c % 4
        blocks = [4 * m + j for m in range(NOWN)]
        xbat = x[b]
        xo = np.ascontiguousarray(xbat.reshape(NBA, 128, 2048)[blocks].reshape(NOWN * 128, 2048))
        posb = np.ascontiguousarray(positions[b].reshape(NBA, 128).T)
        poso = np.ascontiguousarray(positions[b].reshape(NBA, 128)[blocks].T)
        cm = np.zeros((128, 4, 128), np.float32)
        for i in range(4):
            if i < j:
                cm[:, i, :] = 1.0
            elif i == j:
                cm[:, i, :] = tri
        d = dict(shared)
        d.update({"xb": xbat, "xo": xo, "posb": posb, "poso": poso, "cmask": cm.reshape(128, 512)})
        in_maps.append(d)
    return in_maps


def kernel(**inputs):
    in_maps = _prep_inputs(**inputs)
    nc = build_nc()
    res = run_bass_kernel_spmd(nc, in_maps, core_ids=list(range(8)))
    out = np.empty((2, NBA * 128, 2048), np.float32)
    for c in range(8):
        b, j = c // 4, c % 4
        yc = np.asarray(res.results[c]["y"]).reshape(NOWN, 128, 2048)
        for m in range(NOWN):
            blk = 4 * m + j
            out[b, blk * 128:(blk + 1) * 128] = yc[m]
    return out
```
